# Optimizing a Trainium2 kernel written in Bass

```python
import math
import jax, jax.numpy as jnp
from jax import lax
import numpy as np

D_MODEL = 1024
BATCH = 4
SEQ = 4096
DEPTH = 1

N_META = 16
MIX_WIDTH = D_MODEL
ATTN_WIDTH = MIX_WIDTH // 2
POOL_WIDTH = MIX_WIDTH - ATTN_WIDTH
ATTN_HEADS = 4
ATTN_VDIM = ATTN_WIDTH // ATTN_HEADS
ATTN_QKDIM = ATTN_VDIM // 2
POOL_WINDOWS = (2, 4, 8, 16)
POOL_GROUPS = len(POOL_WINDOWS)
POOL_GDIM = POOL_WIDTH // POOL_GROUPS
N_EXPERTS = 16
EC_CAPACITY_FACTOR = 2
D_FF_EXPERT = 2816
Q_BLOCK = 128
EPS = 1e-6
IN_WIDTH = 4 * ATTN_HEADS * ATTN_QKDIM + ATTN_WIDTH + POOL_WIDTH

kernel_name = "hybrid_diffattn_pool_ecmoe_encoder"


def rmsnorm(x, g):
    xf = x.astype(jnp.float32)
    y = xf * lax.rsqrt(jnp.mean(xf * xf, axis=-1, keepdims=True) + EPS)
    return (y * g.astype(jnp.float32)).astype(x.dtype)


def alibi_slopes(n_heads):
    return jnp.array([2.0 ** (-8.0 * (i + 1) / n_heads) for i in range(n_heads)], dtype=jnp.float32)


def diff_attention(q1, q2, k1, k2, v, lam):
    B, L, H, d = q1.shape
    scale = 1.0 / math.sqrt(d)
    slopes = alibi_slopes(H)
    n_blk = -(-L // Q_BLOCK)
    Lp = n_blk * Q_BLOCK
    tr = lambda t: jnp.transpose(t, (0, 2, 1, 3))
    q1, q2, k1, k2, v = tr(q1), tr(q2), tr(k1), tr(k2), tr(v)
    pad = ((0, 0), (0, 0), (0, Lp - L), (0, 0))
    to_blocks = lambda q: jnp.moveaxis(jnp.pad(q, pad).reshape(B, H, n_blk, Q_BLOCK, d), 2, 0)
    q1b, q2b = to_blocks(q1), to_blocks(q2)
    starts = jnp.arange(n_blk, dtype=jnp.int32) * Q_BLOCK
    kpos = jnp.arange(L, dtype=jnp.float32)

    def block(args):
        qa, qb, start = args
        qpos = (start + jnp.arange(Q_BLOCK, dtype=jnp.int32)).astype(jnp.float32)
        dist = jnp.abs(qpos[:, None] - kpos[None, :])
        bias = -slopes[:, None, None] * dist[None]
        s1 = jnp.einsum('bhqd,bhkd->bhqk', qa, k1).astype(jnp.float32) * scale + bias
        s2 = jnp.einsum('bhqd,bhkd->bhqk', qb, k2).astype(jnp.float32) * scale + bias
        att = jax.nn.softmax(s1, axis=-1) - lam * jax.nn.softmax(s2, axis=-1)
        return jnp.einsum('bhqk,bhkv->bhqv', att.astype(v.dtype), v)

    out = lax.map(block, (q1b, q2b, starts))
    out = jnp.moveaxis(out, 0, 2).reshape(B, H, Lp, v.shape[-1])[:, :, :L]
    return jnp.transpose(out, (0, 2, 1, 3))


def multiscale_pool(u, pool_w, pool_scale):
    B, L, _ = u.shape
    uf = u.astype(jnp.float32)
    cs = jnp.concatenate([jnp.zeros((B, 1, POOL_WIDTH), jnp.float32), jnp.cumsum(uf, axis=1)], axis=1)
    t = jnp.arange(L, dtype=jnp.int32)
    parts = []
    for gi, w in enumerate(POOL_WINDOWS):
        sl = slice(gi * POOL_GDIM, (gi + 1) * POOL_GDIM)
        lo = jnp.clip(t - w // 2, 0, L)
        hi = jnp.clip(t + w // 2, 0, L)
        csg = cs[:, :, sl]
        cnt = (hi - lo).astype(jnp.float32)[None, :, None]
        parts.append((csg[:, hi] - csg[:, lo]) / cnt)
    pooled = (jnp.concatenate(parts, axis=-1) - uf).astype(u.dtype)
    pg = pooled.reshape(B, L, POOL_GROUPS, POOL_GDIM)
    y = jnp.einsum('blgc,gcd->blgd', pg, pool_w).reshape(B, L, POOL_WIDTH)
    return y * pool_scale


def expert_choice_moe(h, router_w, w_gate, w_up, w_down):
    B, L, D = h.shape
    C = EC_CAPACITY_FACTOR * L // N_EXPERTS
    logits = jnp.einsum('bld,de->ble', h, router_w).astype(jnp.float32)
    aff = jax.nn.softmax(logits, axis=-1)
    gate, idx = lax.top_k(jnp.swapaxes(aff, 1, 2), C)
    xg = jax.vmap(lambda hb, ib: hb[ib])(h, idx)
    hid = jax.nn.silu(jnp.einsum('becd,edf->becf', xg, w_gate)) * jnp.einsum('becd,edf->becf', xg, w_up)
    y = jnp.einsum('becf,efd->becd', hid, w_down) * gate[..., None].astype(h.dtype)
    return jax.vmap(lambda yb, ib: jnp.zeros((L, D), yb.dtype).at[ib.reshape(-1)].add(yb.reshape(-1, D)))(y, idx)


def setup_inputs(seed: int = 0) -> dict:
    key = jax.random.key(seed)
    ks = jax.random.split(key, 16)
    f32 = jnp.float32
    nrm = lambda k, s, sc: jax.random.normal(k, s, f32) * sc
    return {
        "x": jax.random.normal(ks[0], (BATCH, SEQ, D_MODEL), f32),
        "meta_tokens": nrm(ks[1], (N_META, D_MODEL), 1.0),
        "g_mix": 1.0 + nrm(ks[2], (DEPTH, D_MODEL), 0.02),
        "w_in": nrm(ks[3], (DEPTH, D_MODEL, IN_WIDTH), D_MODEL ** -0.5),
        "lam_vecs": nrm(ks[4], (DEPTH, 4, ATTN_QKDIM), 0.1),
        "subln_gain": 1.0 + nrm(ks[5], (DEPTH, ATTN_VDIM), 0.02),
        "pool_w": nrm(ks[6], (DEPTH, POOL_GROUPS, POOL_GDIM, POOL_GDIM), POOL_GDIM ** -0.5),
        "pool_scale": 1.0 + nrm(ks[7], (DEPTH, POOL_WIDTH), 0.02),
        "w_o": nrm(ks[8], (DEPTH, MIX_WIDTH, D_MODEL), MIX_WIDTH ** -0.5),
        "g_ffn": 1.0 + nrm(ks[9], (DEPTH, D_MODEL), 0.02),
        "router_w": nrm(ks[10], (DEPTH, D_MODEL, N_EXPERTS), D_MODEL ** -0.5),
        "w_gate": nrm(ks[11], (DEPTH, N_EXPERTS, D_MODEL, D_FF_EXPERT), D_MODEL ** -0.5),
        "w_up": nrm(ks[12], (DEPTH, N_EXPERTS, D_MODEL, D_FF_EXPERT), D_MODEL ** -0.5),
        "w_down": nrm(ks[13], (DEPTH, N_EXPERTS, D_FF_EXPERT, D_MODEL), D_FF_EXPERT ** -0.5),
        "g_final": 1.0 + nrm(ks[14], (D_MODEL,), 0.02),
    }


def reference(x, meta_tokens, g_mix, w_in, lam_vecs, subln_gain, pool_w, pool_scale, w_o,
              g_ffn, router_w, w_gate, w_up, w_down, g_final):
    B = x.shape[0]
    meta = jnp.broadcast_to(meta_tokens[None].astype(x.dtype), (B, N_META, D_MODEL))
    h = jnp.concatenate([meta, x], axis=1)
    L = h.shape[1]
    H, d = ATTN_HEADS, ATTN_QKDIM
    qk = H * d
    for l in range(DEPTH):
        lam_init = 0.8 - 0.6 * math.exp(-0.3 * l)
        n = rmsnorm(h, g_mix[l])
        proj = jnp.einsum('bld,de->ble', n, w_in[l])
        q1 = proj[..., 0 * qk:1 * qk].reshape(B, L, H, d)
        q2 = proj[..., 1 * qk:2 * qk].reshape(B, L, H, d)
        k1 = proj[..., 2 * qk:3 * qk].reshape(B, L, H, d)
        k2 = proj[..., 3 * qk:4 * qk].reshape(B, L, H, d)
        v = proj[..., 4 * qk:4 * qk + ATTN_WIDTH].reshape(B, L, H, ATTN_VDIM)
        u = proj[..., 4 * qk + ATTN_WIDTH:]
        lv = lam_vecs[l].astype(jnp.float32)
        lam = jnp.exp(jnp.sum(lv[0] * lv[1])) - jnp.exp(jnp.sum(lv[2] * lv[3])) + lam_init
        a = diff_attention(q1, q2, k1, k2, v, lam)
        a = (rmsnorm(a, subln_gain[l]) * (1.0 - lam_init)).reshape(B, L, ATTN_WIDTH)
        p = multiscale_pool(u, pool_w[l], pool_scale[l])
        mix = jnp.concatenate([a, p], axis=-1)
        h = h + jnp.einsum('blm,md->bld', mix, w_o[l])
        h = h + expert_choice_moe(rmsnorm(h, g_ffn[l]), router_w[l], w_gate[l], w_up[l], w_down[l])
    return rmsnorm(h, g_final)[:, N_META:]
```

```python
from contextlib import ExitStack
import math
import numpy as np
import ml_dtypes
import concourse.bass as bass
import concourse.mybir as mybir
from concourse.bass_utils import run_bass_kernel_spmd

F32 = mybir.dt.float32
BF16 = mybir.dt.bfloat16
I32 = mybir.dt.int32
ALU = mybir.AluOpType
AF = mybir.ActivationFunctionType
AX = mybir.AxisListType

ENGS = ("pe", "dve", "act", "pool", "sp")

D = 1024
L = 4112
NT = 33
LP = NT * 128
NE = 16
CAP = 514
FF = 2816
NF = FF // 128
EPS = 1e-6
LAM_INIT = 0.2
SLOT_TILES = [(0, 2), (2, 128), (130, 128), (258, 128), (386, 128)]
BISECT_ITERS = 26


class Prog:
    def __init__(self, nc):
        self.nc = nc
        self.q = {e: [] for e in ENGS}
        self.cnt = {e: 0 for e in ENGS}
        self.waited = {e: {} for e in ENGS}
        self.last_w = {}
        self.readers = {}
        self.dcnt = {}
        self.sems = {}

    def sem(self, name):
        if name not in self.sems:
            self.sems[name] = self.nc.alloc_semaphore(name="s_" + name)
        return self.sems[name]

    def _deps(self, eng, reads, writes, skip_self):
        deps = set()
        for k in reads:
            if k in self.last_w:
                deps.add(self.last_w[k])
        for k in writes:
            if k in self.last_w:
                deps.add(self.last_w[k])
            deps.update(self.readers.get(k, ()))
        best = {}
        for (s, v) in deps:
            if skip_self and s == eng:
                continue
            if v > best.get(s, 0):
                best[s] = v
        waits = []
        for s in sorted(best):
            v = best[s]
            if self.waited[eng].get(s, 0) >= v:
                continue
            self.waited[eng][s] = v
            waits.append((s, v))
        return waits

    def _commit(self, me, reads, writes):
        for k in writes:
            self.last_w[k] = me
            self.readers[k] = []
        for k in reads:
            if k not in writes:
                self.readers.setdefault(k, []).append(me)

    def op(self, eng, fn, reads=(), writes=(), skip_self=False):
        waits = self._deps(eng, reads, writes, skip_self)
        self.cnt[eng] += 1
        me = (eng, self.cnt[eng])
        self.q[eng].append((waits, fn, eng, 1))
        self._commit(me, reads, writes)
        return me

    def dma(self, eng, fn, semname, reads=(), writes=()):
        waits = self._deps(eng, reads, writes, False)
        semname = "d_" + semname
        self.dcnt[semname] = self.dcnt.get(semname, 0) + 16
        me = (semname, self.dcnt[semname])
        self.q[eng].append((waits, fn, semname, 16))
        self._commit(me, reads, writes)
        return me

    def finalize_group(self, semname, keys):
        me = ("d_" + semname, self.dcnt["d_" + semname])
        for k in keys:
            self.last_w[k] = me

    def barrier(self):
        snap = {e: self.cnt[e] for e in ENGS if self.cnt[e] > 0}
        snap.update(self.dcnt)
        for eng in ENGS:
            waits = []
            for s in sorted(snap):
                v = snap[s]
                if s == eng or self.waited[eng].get(s, 0) >= v:
                    continue
                self.waited[eng][s] = v
                waits.append((s, v))
            if waits:
                self.q[eng].append((waits, None, None, 0))

    def wait_all(self, eng, keys):
        waits = self._deps(eng, keys, keys, False)
        if waits:
            self.q[eng].append((waits, None, None, 0))

    def emit(self):
        nc = self.nc
        for e in ENGS:
            self.sem(e)
        for s in list(self.dcnt):
            self.sem(s)
        prog = self

        def run(engname, engine):
            for waits, fn, incsem, incv in prog.q[engname]:
                for (s, v) in waits:
                    engine.wait_ge(prog.sem(s), v)
                if fn is not None:
                    inst = fn(engine)
                    inst.then_inc(prog.sem(incsem), incv)

        with nc.Block() as block:
            @block.tensor
            def _(eng):
                run("pe", eng)

            @block.vector
            def _(eng):
                run("dve", eng)

            @block.scalar
            def _(eng):
                run("act", eng)

            @block.gpsimd
            def _(eng):
                run("pool", eng)

            @block.sync
            def _(eng):
                run("sp", eng)


def build(debug=False):
    nc = bass.Bass("TRN2", target_bir_lowering=False)
    P = Prog(nc)

    def din(name, shape, dt=F32):
        return nc.dram_tensor(name, list(shape), dt, kind="ExternalInput").ap()

    hx = din("hx", [LP, D])
    w_in = din("w_in", [D, 2048])
    w_o = din("w_o", [D, D])
    router_w = din("router_w", [D, NE])
    pool_w = din("pool_w", [4, 128, 128])
    w_gate = din("w_gate", [NE, D, FF])
    w_up = din("w_up", [NE, D, FF])
    w_down = din("w_down", [NE, FF, D])
    gmix_col_d = din("gmix_col", [128, 8])
    gffn_col_d = din("gffn_col", [128, 8])
    gfin_rep_d = din("gfin_rep", [128, D])
    lam_rep_d = din("lam_rep", [128, 256])
    subln_col_d = din("subln_col", [128, 1])
    pscale_col_d = din("pscale_col", [128, 4])
    identf_d = din("identf", [128, 128])
    identb_d = din("identb", [128, 128], BF16)
    onesf_d = din("onesf", [128, 128])
    onesb_d = din("onesb", [128, 128], BF16)
    onesl_d = din("onesl", [128, 128], BF16)
    ltri_d = din("ltri", [128, 128])
    rstrip_d = din("rstrip", [128, 512])
    astrip_d = din("astrip", [128, 896])
    iota_d = din("iota_slots", [128, 640])
    tokidx_d = din("tokidx", [128, NT])
    toka_d = din("toka", [128, NT])
    tokb_d = din("tokb", [128, NT])
    validm_d = din("validm", [128, NT])
    vm1_d = din("validm1", [128, NT])
    corr_d = din("pool_corr", [128, 4, 16])

    out = nc.dram_tensor("out", [4096, D], F32, kind="ExternalOutput").ap()
    acc0 = nc.dram_tensor("acc0", [LP, 512], F32, kind="Internal").ap()
    acc1 = nc.dram_tensor("acc1", [LP, 512], F32, kind="Internal").ap()
    accs = [acc0, acc1]
    n2d = nc.dram_tensor("n2d", [LP, D], BF16, kind="Internal").ap()
    if debug:
        dbg_h1 = nc.dram_tensor("dbg_h1", [LP, D], F32, kind="ExternalOutput").ap()
        dbg_aff = nc.dram_tensor("dbg_aff", [128, NT * NE], F32, kind="ExternalOutput").ap()
        dbg_idx = nc.dram_tensor("dbg_idx", [128, NE * 5 * 2], F32, kind="ExternalOutput").ap()

    def MM(o, lhsT, rhs, start, stop, r, w):
        P.op("pe", lambda e: e.matmul(o, lhsT=lhsT, rhs=rhs, start=start, stop=stop), r, w, skip_self=True)

    def TR(o, i, ident, r, w):
        P.op("pe", lambda e: e.transpose(out=o, in_=i, identity=ident), r, w, skip_self=True)

    def ACT(o, i, func, r, w, bias=None, scale=None, accum=None):
        kw = {}
        if bias is not None:
            kw["bias"] = bias
        if scale is not None:
            kw["scale"] = scale
        if accum is not None:
            kw["accum_out"] = accum
        P.op("act", lambda e: e.activation(out=o, in_=i, func=func, **kw), r, w)

    def TS(eng, o, i0, s1, s2, op0, op1, r, w):
        if op1 is None:
            P.op(eng, lambda e: e.tensor_scalar(out=o, in0=i0, scalar1=s1, scalar2=None, op0=op0), r, w)
        else:
            P.op(eng, lambda e: e.tensor_scalar(out=o, in0=i0, scalar1=s1, scalar2=s2, op0=op0, op1=op1), r, w)

    def TT(eng, o, i0, i1, op, r, w):
        P.op(eng, lambda e: e.tensor_tensor(out=o, in0=i0, in1=i1, op=op), r, w)

    def STT(o, i0, sc, i1, op0, op1, r, w):
        P.op("dve", lambda e: e.scalar_tensor_tensor(out=o, in0=i0, scalar=sc, in1=i1, op0=op0, op1=op1), r, w)

    def CP(eng, o, i, r, w):
        if eng == "act":
            P.op("act", lambda e: e.activation(out=o, in_=i, func=AF.Copy), r, w)
        else:
            P.op(eng, lambda e: e.tensor_copy(out=o, in_=i), r, w)

    def DMA(eng, o, i, sem, r, w):
        P.dma(eng, lambda e: e.dma_start(out=o, in_=i), sem, r, w)

    with ExitStack() as g:
        def sb(name, shape, dt=F32, es=g):
            return es.enter_context(nc.sbuf_tensor("sb_" + name, list(shape), dt))

        def ps(name, shape, dt=F32, es=g):
            return es.enter_context(nc.psum_tensor("ps_" + name, list(shape), dt))

        s1 = ExitStack()
        identf = sb("identf", [128, 128])
        identb = sb("identb", [128, 128], BF16)
        onesf = sb("onesf", [128, 128])
        onesb = sb("onesb", [128, 128], BF16)
        ltri = sb("ltri", [128, 128])
        iota_s = sb("iota_s", [128, 640])
        tokidx = sb("tokidx", [128, NT])
        toka = sb("toka", [128, NT])
        tokb = sb("tokb", [128, NT])
        gffn_col = sb("gffn_col", [128, 8])
        epsc = sb("epsc", [128, 1])
        neglam = sb("neglam", [128, 1])
        gcol8 = sb("gcol8", [128, 1])
        lam2 = sb("lam2", [128, 2])
        aff_all = sb("aff_all", [128, NT, NE])

        onesl = sb("onesl", [128, 128], BF16, s1)
        rstrip = sb("rstrip", [128, 512], F32, s1)
        astrip = sb("astrip", [128, 896], F32, s1)
        validm = sb("validm", [128, NT], F32, s1)
        vm1 = sb("vm1", [128, NT], F32, s1)
        corr = sb("corr", [128, 4, 16], F32, s1)
        gmix_col = sb("gmix_col", [128, 8], F32, s1)
        lam_rep = sb("lam_rep", [128, 256], F32, s1)
        subln_col = sb("subln_col", [128, 1], F32, s1)
        pscale_col = sb("pscale_col", [128, 4], F32, s1)
        rw_f = sb("rw_f", [128, 8, NE], F32, s1)
        rw_g = sb("rw_g", [128, 8, NE], F32, s1)
        pw_f = sb("pw_f", [128, 4, 128], F32, s1)
        pw_b = sb("pw_b", [128, 4, 128], BF16, s1)
        lamt = sb("lamt", [128, 2, 64], F32, s1)
        consts = [(identf, identf_d, "identf"), (identb, identb_d, "identb"), (onesf, onesf_d, "onesf"),
                  (onesb, onesb_d, "onesb"), (onesl, onesl_d, "onesl"), (ltri, ltri_d, "ltri"), (rstrip, rstrip_d, "rstrip"),
                  (astrip, astrip_d, "astrip"), (iota_s, iota_d, "iota_s"), (tokidx, tokidx_d, "tokidx"), (toka, toka_d, "toka"), (tokb, tokb_d, "tokb"),
                  (validm, validm_d, "validm"), (vm1, vm1_d, "vm1"), (corr, corr_d, "corr"),
                  (gmix_col, gmix_col_d, "gmix_col"), (gffn_col, gffn_col_d, "gffn_col"),
                  (lam_rep, lam_rep_d, "lam_rep"), (subln_col, subln_col_d, "subln_col"),
                  (pscale_col, pscale_col_d, "pscale_col"),
                  (rw_f, router_w.rearrange("(c p) n -> p c n", p=128), "rw_f"),
                  (pw_f, pool_w.rearrange("g c d -> c g d"), "pw_f")]
        for i, (t, d, k) in enumerate(consts):
            DMA("act" if i % 2 else "sp", t[:], d, "const", [], [k])
        P.finalize_group("const", [k for (_, _, k) in consts])

        P.op("pool", lambda e: e.memset(epsc[:], EPS), [], ["epsc"])
        lam4 = lam_rep[:].rearrange("p (i w k) -> p i w k", i=2, w=2)
        TT("dve", lamt[:], lam4[:, :, 0, :], lam4[:, :, 1, :], ALU.mult, ["lam_rep"], ["lamt"])
        P.op("dve", lambda e: e.tensor_reduce(out=lam2[:], in_=lamt[:], axis=AX.X, op=ALU.add), ["lamt"], ["lam2"])
        ACT(lam2[:], lam2[:], AF.Exp, ["lam2"], ["lam2"])
        TT("dve", neglam[:], lam2[:, 1:2], lam2[:, 0:1], ALU.subtract, ["lam2"], ["neglam"])
        TS("dve", neglam[:], neglam[:], -LAM_INIT, None, ALU.add, None, ["neglam"], ["neglam"])
        TS("dve", gcol8[:], subln_col[:], 1.0 - LAM_INIT, None, ALU.mult, None, ["subln_col"], ["gcol8"])
        CP("dve", pw_b[:], pw_f[:], ["pw_f"], ["pw_b"])
        for c in range(8):
            TS("dve", rw_g[:, c, :], rw_f[:, c, :], gffn_col[:, c:c + 1], None, ALU.mult, None,
               ["rw_f", "gffn_col"], ["rw_g"])

        gmix_b8 = gmix_col[:, :].unsqueeze(2).to_broadcast([128, 8, 128])
        with s1:
            KT = sb("KT", [128, 4, LP], BF16, s1)
            V = sb("V", [128, NT, 512], BF16, s1)
            pT = sb("pT", [128, 4, LP], BF16, s1)
            xt = [sb(f"xt{i}", [128, D], F32, s1) for i in range(2)]
            nt = sb("nt", [128, D], F32, s1)
            nT = sb("nT", [128, 8, 512], BF16, s1)
            sqj = sb("sqj", [128, D], BF16, s1)
            ssq = sb("ssq", [128, 1], F32, s1)
            rstd = sb("rstd", [128, 1], F32, s1)
            RB = [ps(f"RB{i}", [128, 512], F32, s1) for i in range(4)]
            psO = [ps(f"psO{i}", [128, 512], F32, s1) for i in range(4)]
            xcnt = [0]
            rbc = [0]

            def rb():
                i = rbc[0] % 4
                rbc[0] += 1
                return RB[i], f"RB{i}"

            ntb = [nt[:, 0:512].bitcast(BF16), nt[:, 512:1024].bitcast(BF16)]
            ssqs = [sb(f"ssqs{i}", [128, 1], F32, s1) for i in range(2)]
            rstds = [sb(f"rstds{i}", [128, 1], F32, s1) for i in range(2)]
            ncnt = [0]

            def norm_tile(j, ntok_off):
                sl = xcnt[0] % 2
                xcnt[0] += 1
                nb = ncnt[0] % 2
                ncnt[0] += 1
                nk = f"ntb{nb}"
                DMA("sp", xt[sl][:], hx[j * 128:(j + 1) * 128, :], f"xt{sl}", [], [f"xt{sl}"])
                ACT(sqj[:], xt[sl][:], AF.Square, [f"xt{sl}"], ["sqj", f"ssqs{nb}"], accum=ssqs[nb][:])
                ACT(rstds[nb][:], ssqs[nb][:], AF.Ln, [f"ssqs{nb}", "epsc"], [f"rstds{nb}"], bias=epsc[:], scale=1.0 / D)
                ACT(rstds[nb][:], rstds[nb][:], AF.Exp, [f"rstds{nb}"], [f"rstds{nb}"], scale=-0.5)
                TS("dve", ntb[nb], xt[sl][:], rstds[nb][:], None, ALU.mult, None, [f"xt{sl}", f"rstds{nb}"], [nk, "nt"])
                bank, bk = rb()
                pv = bank[:].bitcast(BF16).rearrange("p (c n) -> p c n", c=8)
                for cc in range(8):
                    TR(pv[:, cc, :], ntb[nb][:, cc * 128:(cc + 1) * 128], identb[:], [nk, "identb"], [bk])
                CP("act" if nb else "dve", nT[:, :, ntok_off:ntok_off + 128], pv, [bk], ["nT"])

            castc = [0]

            def load_cast(dst, dkey, src_view, col0, ncols, stg, stgk, fold=None):
                for i in range(ncols // 256):
                    sl = i % 2
                    sv = stg[sl][:].rearrange("p (c n) -> p c n", c=8)
                    DMA("sp", sv, src_view[:, :, col0 + 256 * i: col0 + 256 * (i + 1)], stgk[sl], [], [stgk[sl]])
                    if fold is None:
                        CP("act" if castc[0] % 2 else "dve", dst[:, :, 256 * i:256 * (i + 1)], sv, [stgk[sl]], [dkey])
                        castc[0] += 1
                    else:
                        for c in range(8):
                            if c % 2:
                                P.op("act", lambda e, c=c, i=i, sv=sv: e.activation(
                                    out=dst[:, c, 256 * i:256 * (i + 1)], in_=sv[:, c, :], func=AF.Copy,
                                    scale=fold[:, c:c + 1]), [stgk[sl], "gmix_col"], [dkey])
                            else:
                                TS("dve", dst[:, c, 256 * i:256 * (i + 1)], sv[:, c, :], fold[:, c:c + 1], None, ALU.mult,
                                   None, [stgk[sl], "gmix_col"], [dkey])

            w_in_v = w_in.rearrange("(c p) n -> p c n", p=128)
            with ExitStack() as sa:
                wkvu = sb("wkvu", [128, 8, 1536], BF16, sa)
                stgA = [sb(f"stgA{i}", [128, 2048], F32, sa) for i in range(2)]
                UT = [sb(f"UT{i}", [128, 4, 528], F32, sa) for i in range(2)]
                tA = sb("tA", [128, 528], F32, sa)
                tB = sb("tB", [128, 528], F32, sa)
                tC = sb("tC", [128, 528], F32, sa)
                pooled = sb("pooled", [128, 512], BF16, sa)
                load_cast(wkvu, "wkvu", w_in_v, 512, 1536, stgA, ["stgA0", "stgA1"], fold=gmix_col)
                P.op("pool", lambda e: e.memset(UT[0][:], 0.0), [], ["UT0"])
                P.op("pool", lambda e: e.memset(UT[1][:], 0.0), [], ["UT1"])
                nchunks = 9
                psrot = [0]

                def nextbank():
                    b = psrot[0] % 8
                    psrot[0] += 1
                    if b < 4:
                        return RB[b], f"RB{b}"
                    return psO[b - 4], f"psO{b - 4}"

                def do_pool(c):
                    u = UT[c % 2]
                    uk = f"UT{c % 2}"
                    nq = 512 if c < 8 else 128
                    E = "dve"
                    for gi, w in enumerate((2, 4, 8, 16)):
                        ug = u[:, gi, :]
                        if gi == 0:
                            TT(E, tC[:, 0:512], ug[:, 7:519], ug[:, 8:520], ALU.add, [uk], ["tC"])
                        elif gi == 1:
                            TT(E, tA[:, 0:526], ug[:, 0:526], ug[:, 1:527], ALU.add, [uk], ["tA"])
                            TT(E, tC[:, 0:512], tA[:, 6:518], tA[:, 8:520], ALU.add, ["tA"], ["tC"])
                        elif gi == 2:
                            TT(E, tA[:, 0:526], ug[:, 0:526], ug[:, 1:527], ALU.add, [uk], ["tA"])
                            TT(E, tB[:, 0:524], tA[:, 0:524], tA[:, 2:526], ALU.add, ["tA"], ["tB"])
                            TT(E, tC[:, 0:512], tB[:, 4:516], tB[:, 8:520], ALU.add, ["tB"], ["tC"])
                        else:
                            TT(E, tA[:, 0:526], ug[:, 0:526], ug[:, 1:527], ALU.add, [uk], ["tA"])
                            TT(E, tB[:, 0:524], tA[:, 0:524], tA[:, 2:526], ALU.add, ["tA"], ["tB"])
                            TT(E, tA[:, 0:520], tB[:, 0:520], tB[:, 4:524], ALU.add, ["tB"], ["tA"])
                            TT(E, tC[:, 0:512], tA[:, 0:512], tA[:, 8:520], ALU.add, ["tA"], ["tC"])
                        win = tC[:, 0:512]
                        wk = "tC"
                        if c == 0:
                            TT(E, tC[:, 0:8], tC[:, 0:8], corr[:, gi, 0:8], ALU.mult, [wk, "corr"], [wk])
                        if c == 8:
                            TT(E, tC[:, 8:16], tC[:, 8:16], corr[:, gi, 8:16], ALU.mult, [wk, "corr"], [wk])
                        STT(pooled[:, :], win, 1.0 / w, ug[:, 8:520], ALU.mult, ALU.subtract, [wk, uk], ["pooled"])
                        bank, bk = nextbank()
                        MM(bank[:, 0:nq], pw_b[:, gi, :], pooled[:, 0:nq], True, True, ["pw_b", "pooled"], [bk])
                        P.op("act", lambda e, gi=gi, c=c, nq=nq, bank=bank: e.activation(
                            out=pT[:, gi, c * 512:c * 512 + nq], in_=bank[:, 0:nq], func=AF.Copy,
                            scale=pscale_col[:, gi:gi + 1]), [bk, "pscale_col"], ["pT"])

                for c in range(nchunks):
                    ntl = 4 if c < 8 else 1
                    nq = ntl * 128
                    for ti in range(ntl):
                        norm_tile(c * 4 + ti, ti * 128)
                    for fc in range(4):
                        bank, bk = nextbank()
                        for kc in range(8):
                            MM(bank[:, 0:nq], wkvu[:, kc, fc * 128:(fc + 1) * 128], nT[:, kc, 0:nq], kc == 0, kc == 7,
                               ["wkvu", "nT"], [bk])
                        CP("act", KT[:, fc, c * 512:c * 512 + nq], bank[:, 0:nq], [bk], ["KT"])
                    u = UT[c % 2]
                    uk = f"UT{c % 2}"
                    if c >= 2:
                        P.op("pool", lambda e, u=u: e.memset(u[:], 0.0), [], [uk])
                    for gi in range(4):
                        bank, bk = nextbank()
                        for kc in range(8):
                            MM(bank[:, 0:nq], wkvu[:, kc, 1024 + gi * 128:1024 + (gi + 1) * 128], nT[:, kc, 0:nq],
                               kc == 0, kc == 7, ["wkvu", "nT"], [bk])
                        CP("act" if gi % 2 else "dve", u[:, gi, 8:8 + nq], bank[:, 0:nq], [bk], [uk])
                    for ti in range(ntl):
                        bank, bk = nextbank()
                        for kc in range(8):
                            MM(bank[:, 0:512], nT[:, kc, ti * 128:(ti + 1) * 128], wkvu[:, kc, 512:1024], kc == 0, kc == 7,
                               ["wkvu", "nT"], [bk])
                        CP("act" if ti % 2 else "dve", V[:, c * 4 + ti, :], bank[:, 0:512], [bk], ["V"])
                    if c >= 1:
                        up = UT[(c - 1) % 2]
                        upk = f"UT{(c - 1) % 2}"
                        CP("dve", up[:, :, 520:528], u[:, :, 8:16], [uk], [upk])
                        CP("dve", u[:, :, 0:8], up[:, :, 512:520], [upk], [uk])
                        do_pool(c - 1)
                do_pool(nchunks - 1)

            P.barrier()
            with ExitStack() as sq_:
                wq = sb("wq", [128, 8, 512], BF16, sq_)
                wo = sb("wo", [128, 8, D], BF16, sq_)
                with ExitStack() as stmp:
                    stgB = [sb(f"stgB{i}", [128, 2048], F32, stmp) for i in range(2)]
                    load_cast(wq, "wq", w_in_v, 0, 512, stgB, ["stgB0", "stgB1"], fold=gmix_col)
                    load_cast(wo, "wo", w_o.rearrange("(c p) n -> p c n", p=128), 0, 1024, stgB, ["stgB0", "stgB1"])
                P.barrier()
                NTMP = 3
                NPT = 5
                PD = 3
                QT = sb("QT", [128, 4, 2, 512], BF16, sq_)
                P.op("pool", lambda e: e.memset(QT[:], 0.0), [], ["QT"])
                tmp = [sb(f"tmp{i}", [128, 512], F32, sq_) for i in range(NTMP)]
                PT = [sb(f"PT{i}", [128, 512], BF16, sq_) for i in range(NPT)]
                eA = sb("eA", [128, 512], F32, sq_)
                eB = sb("eB", [128, 512], F32, sq_)
                eD = sb("eD", [128, 512], F32, sq_)
                eS = sb("eS", [128, 512], F32, sq_)
                eR = eS
                aTs = [sb(f"aT{i}", [128, 4, 512], BF16, sq_) for i in range(2)]
                h1t = sb("h1t", [128, D], F32, sq_)
                n2f = nt
                n2b = [sqj]
                n2T = sb("n2T", [128, 8, 128], F32, sq_)
                ssq2 = sb("ssq2", [128, 1], F32, sq_)
                rstd2 = sb("rstd2", [128, 1], F32, sq_)
                lg = sb("lg", [128, NE], F32, sq_)
                mx = sb("mx", [128, 1], F32, sq_)
                se = sb("se", [128, 1], F32, sq_)
                tcount = [0]
                hcount = [0]

                pending_b = [None]

                def step_b():
                    if pending_b[0] is not None:
                        try:
                            next(pending_b[0])
                        except StopIteration:
                            pending_b[0] = None

                def drain_b():
                    while pending_b[0] is not None:
                        step_b()

                def phase_b(c, q0, ntl, aT, aTk):
                    for ti in range(ntl):
                        j = c * 4 + ti
                        tok = slice(ti * 128, (ti + 1) * 128)
                        sl = xcnt[0] % 2
                        xcnt[0] += 1
                        DMA("sp", xt[sl][:], hx[j * 128:(j + 1) * 128, :], f"xt{sl}", [], [f"xt{sl}"])
                        for half in range(2):
                            bank, bk = rb()
                            for mc in range(8):
                                if mc < 4:
                                    lh = aT[:, mc, tok]
                                    rk = aTk
                                else:
                                    lh = pT[:, mc - 4, q0 + ti * 128:q0 + (ti + 1) * 128]
                                    rk = "pT"
                                MM(bank[:, :], lh, wo[:, mc, half * 512:(half + 1) * 512],
                                   mc == 0, mc == 7, [rk, "wo"], [bk])
                            TT("dve", h1t[:, half * 512:(half + 1) * 512], bank[:, :], xt[sl][:, half * 512:(half + 1) * 512],
                               ALU.add, [bk, f"xt{sl}"], ["h1t"])
                            yield
                        for half in range(2):
                            DMA("sp", accs[half][j * 128:(j + 1) * 128, :], h1t[:, half * 512:(half + 1) * 512],
                                f"h1st{half}", ["h1t"], [f"acc_init{half}"])
                        if debug:
                            DMA("sp", dbg_h1[j * 128:(j + 1) * 128, :], h1t[:], "dbgh1", ["h1t"], ["dbg_h1"])
                        ACT(sqj[:], h1t[:], AF.Square, ["h1t"], ["sqj", "ssq2"], accum=ssq2[:])
                        ACT(rstd2[:], ssq2[:], AF.Ln, ["ssq2", "epsc"], ["rstd2"], bias=epsc[:], scale=1.0 / D)
                        ACT(rstd2[:], rstd2[:], AF.Exp, ["rstd2"], ["rstd2"], scale=-0.5)
                        yield
                        TS("dve", n2f[:], h1t[:], rstd2[:], None, ALU.mult, None, ["h1t", "rstd2", "ntb0", "ntb1"], ["nt", "ntb0", "ntb1"])
                        yield
                        CP("act", n2b[0][:], n2f[:], ["nt"], ["sqj"])
                        DMA("sp", n2d[j * 128:(j + 1) * 128, :], n2b[0][:], "n2st0", ["sqj"], ["n2d0"])
                        for hb in range(2):
                            bank, bk = rb()
                            pv = bank[:].rearrange("p (c n) -> p c n", c=4)
                            for cc in range(4):
                                c8 = hb * 4 + cc
                                TR(pv[:, cc, :], n2f[:, c8 * 128:(c8 + 1) * 128], identf[:], ["nt", "identf"], [bk])
                            CP("act" if hb else "dve", n2T[:, hb * 4:hb * 4 + 4, :], pv, [bk], ["n2T"])
                            yield
                        bank, bk = rb()
                        for cc in range(8):
                            MM(bank[:, 0:NE], n2T[:, cc, :], rw_g[:, cc, :], cc == 0, cc == 7, ["n2T", "rw_g"], [bk])
                        P.op("dve", lambda e, bank=bank: e.tensor_reduce(out=mx[:], in_=bank[:, 0:NE], axis=AX.X, op=ALU.max),
                             [bk], ["mx"])
                        TS("dve", mx[:], mx[:], -1.0, None, ALU.mult, None, ["mx"], ["mx"])
                        ACT(lg[:], bank[:, 0:NE], AF.Exp, [bk, "mx"], ["lg", "se"], bias=mx[:], accum=se[:])
                        yield
                        P.op("dve", lambda e: e.reciprocal(out=se[:], in_=se[:]), ["se"], ["se"])
                        TS("dve", lg[:], lg[:], se[:], None, ALU.mult, None, ["lg", "se"], ["lg"])
                        STT(aff_all[:, j, :], lg[:], validm[:, j:j + 1], vm1[:, j:j + 1].to_broadcast([128, NE]),
                            ALU.mult, ALU.add, ["lg", "validm", "vm1"], ["aff_all"])
                        yield

                for c in range(9):
                    ntl = 4 if c < 8 else 1
                    nq = ntl * 128 if c < 8 else L - 8 * 512
                    q0 = c * 512
                    for ti in range(ntl):
                        norm_tile(c * 4 + ti, ti * 128)
                    for fc in range(4):
                        bank, bk = rb()
                        for kc in range(8):
                            MM(bank[:, 0:nq], wq[:, kc, fc * 128:(fc + 1) * 128], nT[:, kc, 0:nq], kc == 0, kc == 7,
                               ["wq", "nT"], [bk])
                        ACT(QT[0:64, fc, 0, 0:nq], bank[0:64, 0:nq], AF.Copy, [bk], ["QT"], scale=0.125)
                        ACT(QT[64:128, fc, 1, 0:nq], bank[64:128, 0:nq], AF.Copy, [bk, "QT"], ["QT"], scale=0.125)
                    tiles = []
                    for h in range(4):
                        m = 2.0 ** (-2.0 * (h + 1))
                        for mp in range(2):
                            kbs = []
                            for kb in range(NT):
                                k0 = kb * 128
                                kp = 128 if kb < 32 else 16
                                dist = max(0, k0 - (q0 + nq - 1), q0 - (k0 + kp - 1))
                                if dist * m > 32.0:
                                    continue
                                kbs.append(kb)
                            for ii, kb in enumerate(kbs):
                                tiles.append(dict(h=h, mp=mp, kb=kb, first=(ii == 0), last=(ii == len(kbs) - 1), m=m))
                    deferred = []

                    def emit_S(T, nq=nq, q0=q0):
                        h, mp, kb, m = T["h"], T["mp"], T["kb"], T["m"]
                        hp, base = h // 2, 64 * (h % 2)
                        fcq = mp * 2 + hp
                        k0 = kb * 128
                        kp = 128
                        t = tcount[0]
                        tcount[0] += 1
                        T["t"] = t
                        sS, sk = rb()
                        tm, tk = tmp[t % NTMP], f"tmp{t % NTMP}"
                        pt, pk = PT[t % NPT], f"PT{t % NPT}"
                        MM(sS[0:kp, 0:nq], KT[:, fcq, k0:k0 + kp], QT[:, fcq, h % 2, 0:nq],
                           True, True, ["KT", "QT"], [sk])
                        Dd = q0 - k0
                        if k0 + kp - 1 < q0:
                            STT(tm[0:kp, 0:nq], rstrip[0:kp, 0:nq], -m, sS[0:kp, 0:nq], ALU.mult, ALU.add,
                                ["rstrip", sk], [tk])
                            cb = -m * Dd
                        elif k0 >= q0 + nq:
                            STT(tm[0:kp, 0:nq], rstrip[0:kp, 0:nq], m, sS[0:kp, 0:nq], ALU.mult, ALU.add,
                                ["rstrip", sk], [tk])
                            cb = m * Dd
                        else:
                            STT(tm[0:kp, 0:nq], astrip[0:kp, Dd + 384:Dd + 384 + nq], m, sS[0:kp, 0:nq],
                                ALU.mult, ALU.add, ["astrip", sk], [tk])
                            cb = 0.0
                        ACT(pt[0:kp, 0:nq], tm[0:kp, 0:nq], AF.Exp, [tk], [pk], bias=(float(cb) if cb != 0.0 else None))

                    aT = aTs[c % 2]
                    aTk = f"aT{c % 2}"

                    seqc = [0]

                    def defer(time, cls, fn, writes_eD=False, mp=None):
                        seqc[0] += 1
                        deferred.append(dict(time=time, cls=cls, seq=seqc[0], fn=fn, weD=writes_eD, mp=mp))
                        deferred.sort(key=lambda d: (d["time"], d["seq"]))

                    def run_item(d):
                        if d not in deferred:
                            return
                        for o in [o for o in deferred if o is not d and ((o["cls"] == d["cls"] and o["seq"] < d["seq"])
                                                                       or (d["weD"] and o["cls"] == "tail"))]:
                            run_item(o)
                        if d in deferred:
                            deferred.remove(d)
                            d["fn"]()

                    def run_deferred(it, flush=False):
                        while deferred and (flush or deferred[0]["time"] <= it):
                            run_item(deferred[0])

                    def start_tail(h, it, nq=nq, aT=aT, aTk=aTk):
                        bankbox = {}

                        def stC():
                            ACT(eS[:, 0:nq], eD[:, 0:nq], AF.Square, ["eD"], ["eS"])

                        def stD():
                            bank, bk = rb()
                            bankbox["b"] = (bank, bk)
                            MM(bank[:, 0:nq], onesf[:], eS[:, 0:nq], True, True, ["onesf", "eS"], [bk])
                            ACT(eR[:, 0:nq], bank[:, 0:nq], AF.Ln, [bk, "epsc"], ["eS"], bias=epsc[:], scale=1.0 / 128)

                        def stE():
                            ACT(eR[:, 0:nq], eR[:, 0:nq], AF.Exp, ["eS"], ["eS"], scale=-0.5)

                        def stF():
                            STT(aT[:, h, 0:nq], eD[:, 0:nq], gcol8[:], eR[:, 0:nq], ALU.mult, ALU.mult,
                                ["eD", "gcol8", "eS"], [aTk])

                        defer(it + 2, "tail", stC)
                        defer(it + 4, "tail", stD)
                        defer(it + 6, "tail", stE)
                        defer(it + 9, "tail", stF)

                    def start_norm(h, mp, it, nq=nq):
                        def stA():
                            ACT(eA[:, 0:nq], psO[2 + mp][:, 0:nq], AF.Ln, [f"psO{2 + mp}"], ["eA"])
                            ACT(eA[:, 0:nq], eA[:, 0:nq], AF.Exp, ["eA"], ["eA"], scale=-1.0)

                        def stB():
                            if mp == 0:
                                TT("dve", eB[:, 0:nq], psO[0][:, 0:nq], eA[:, 0:nq], ALU.mult, ["psO0", "eA"], ["eB"])
                            else:
                                TT("dve", eA[:, 0:nq], psO[1][:, 0:nq], eA[:, 0:nq], ALU.mult, ["psO1", "eA"], ["eA"])
                                STT(eD[:, 0:nq], eA[:, 0:nq], neglam[:], eB[:, 0:nq], ALU.mult, ALU.add,
                                    ["eA", "neglam", "eB"], ["eD"])
                                start_tail(h, max(it + 8, curit[0]))

                        defer(it + 5, "norm", stA, mp=mp)
                        defer(it + 8, "norm", stB, writes_eD=(mp == 1), mp=mp)

                    curit = [0]

                    def emit_AV(T, it, nq=nq):
                        h, mp, kb = T["h"], T["mp"], T["kb"]
                        kp = 128
                        t = T["t"]
                        pt, pk = PT[t % NPT], f"PT{t % NPT}"
                        if T["first"]:
                            for d in [d for d in deferred if d["cls"] == "norm" and d["mp"] == mp]:
                                run_item(d)
                        MM(psO[mp][:, 0:nq], V[0:kp, kb, h * 128:(h + 1) * 128], pt[0:kp, 0:nq],
                           T["first"], T["last"], ["V", pk], [f"psO{mp}"])
                        MM(psO[2 + mp][:, 0:nq], (onesl if kb == NT - 1 else onesb)[0:kp, :], pt[0:kp, 0:nq],
                           T["first"], T["last"], ["onesb", "onesl", pk], [f"psO{2 + mp}"])
                        if T["last"]:
                            start_norm(h, mp, it)

                    ntile = len(tiles)
                    for it in range(ntile + PD):
                        if it < ntile:
                            emit_S(tiles[it])
                        curit[0] = it
                        if it >= PD:
                            emit_AV(tiles[it - PD], it)
                        run_deferred(it)
                        if it % 3 == 2:
                            step_b()
                    run_deferred(10 ** 9, flush=True)

                    drain_b()
                    pending_b[0] = phase_b(c, q0, ntl, aT, aTk)
                drain_b()
        P.barrier()
        with ExitStack() as s2:
            msk = sb("msk", [128, NE, NT], F32, s2)
            pos = sb("pos", [128, NE, NT], F32, s2)
            lo = sb("lo", [128, NE], F32, s2)
            cmpb = sb("cmpb", [128, NE, NT], F32, s2)
            cntp = sb("cntp", [128, NE], F32, s2)
            gsel = sb("gsel", [128, NE], F32, s2)
            basep = sb("basep", [128, NE], F32, s2)
            onesc = sb("onesc", [128, NT], F32, s2)
            B = [ps(f"B{i}", [128, 512], F32, s2) for i in range(7)]
            BT = ps("BT", [128, 8, 128], BF16, s2)
            affT = aff_all[:].rearrange("p j e -> p e j")

            P.op("dve", lambda e: e.memset(lo[:], 0.0), [], ["lo"])
            P.op("dve", lambda e: e.memset(onesc[:], 1.0), [], ["onesc"])
            for it in range(BISECT_ITERS):
                step = 2.0 ** (-(it + 1))
                STT(cmpb[:], affT, step, lo[:].unsqueeze(2).to_broadcast([128, NE, NT]), ALU.subtract, ALU.is_ge,
                    ["aff_all", "lo"], ["cmpb"])
                P.op("dve", lambda e: e.tensor_reduce(out=cntp[:], in_=cmpb[:], axis=AX.X, op=ALU.add), ["cmpb"], ["cntp"])
                MM(B[0][:, 0:NE], onesf[:], cntp[:], True, True, ["onesf", "cntp"], ["B0"])
                TS("dve", gsel[:], B[0][:, 0:NE], CAP - 0.5, step, ALU.is_ge, ALU.mult, ["B0"], ["gsel"])
                TT("dve", lo[:], lo[:], gsel[:], ALU.add, ["lo", "gsel"], ["lo"])
            TT("dve", msk[:], affT, lo[:].unsqueeze(2).to_broadcast([128, NE, NT]), ALU.is_ge, ["aff_all", "lo"], ["msk"])
            P.op("dve", lambda e: e.tensor_reduce(out=cntp[:], in_=msk[:], axis=AX.X, op=ALU.add), ["msk"], ["cntp"])
            MM(B[0][:, 0:NE], ltri[:], cntp[:], True, True, ["ltri", "cntp"], ["B0"])
            CP("dve", basep[:], B[0][:, 0:NE], ["B0"], ["basep"])
            for e_ in range(NE):
                P.op("dve", lambda e, e_=e_: e.tensor_tensor_scan(out=pos[:, e_, :], data0=onesc[:], data1=msk[:, e_, :],
                                                                  initial=0.0, op0=ALU.mult, op1=ALU.add),
                     ["onesc", "msk"], ["pos"])
            TT("dve", pos[:], pos[:], msk[:], ALU.subtract, ["pos", "msk"], ["pos"])
            TT("dve", pos[:], pos[:], basep[:].unsqueeze(2).to_broadcast([128, NE, NT]), ALU.add, ["pos", "basep"], ["pos"])
            if debug:
                DMA("sp", dbg_aff, aff_all[:].rearrange("p j e -> p (j e)"), "dbgaff", ["aff_all"], ["dbg_aff"])

            valsP = sb("valsP", [128, NT, 128], BF16, s2)
            gtmp = sb("gtmp", [128, NT], F32, s2)
            Sj = [sb(f"Sj{i}", [128, 514], BF16, s2) for i in range(6)]
            cv = sb("cv", [128, 516], F32, s2)
            idxv = sb("idxv", [128, 5, 5], F32, s2)
            idxf = sb("idxf", [128, 5], F32, s2)
            idxi = [sb(f"idxi{i}", [128, 5], I32, s2) for i in range(2)]
            gat = [sb(f"gat{i}", [128, 5], F32, s2) for i in range(2)]
            xg = sb("xg", [128, 5, D], BF16, s2)
            xgT = sb("xgT", [128, 8, 516], BF16, s2)
            hidT = sb("hidT", [128, NF, 516], BF16, s2)
            Wd = sb("Wd", [128, NF, D], BF16, s2)
            stg = [sb(f"stg{i}", [128, 2048], F32, s2) for i in range(4)]
            stgW = [sb(f"stgW{i}", [128, 1024], F32, s2) for i in range(2)]
            wdc = [0]
            Wg = [sb(f"Wg{i}", [128, 8, 512], BF16, s2) for i in range(2)]
            Wu = [sb(f"Wu{i}", [128, 8, 512], BF16, s2) for i in range(2)]
            sg = [sb(f"sg{i}", [128, 257], F32, s2) for i in range(2)]
            yt = [sb(f"yt{i}", [128, 512], F32, s2) for i in range(5)]
            stgc = [0]
            wpc = [0]
            bankc = [0]
            sgc = [0]
            ytc = [0]
            ssc = [0]
            xg_keys = ["xg0", "xg1", "xg2", "xg3", "xg4"]
            if debug:
                dbgi = sb("dbgi", [128, NE * 5 * 2], F32, s2)

            P.op("pool", lambda e: e.memset(valsP[:], 0.0), [], ["valsP"])
            P.op("pool", lambda e: e.memset(cv[:], 0.0), [], ["cv"])
            P.op("pool", lambda e: e.memset(xg[:], 0.0), [], xg_keys)
            P.op("pool", lambda e: e.memset(hidT[:], 0.0), [], ["hidT"])
            CP("dve", valsP[:, :, 0], toka[:], ["toka", "valsP"], ["valsP"])
            CP("dve", valsP[:, :, 1], tokb[:], ["tokb", "valsP"], ["valsP"])

            def comp_prepare(e_):
                g = aff_all[:, :, e_]
                CP("dve", valsP[:, :, 2], g, ["aff_all", "valsP"], ["valsP"])
                TT("dve", gtmp[:], g, valsP[:, :, 2], ALU.subtract, ["aff_all", "valsP"], ["gtmp"])
                CP("dve", valsP[:, :, 3], gtmp[:], ["gtmp", "valsP"], ["valsP"])
                TT("dve", gtmp[:], gtmp[:], valsP[:, :, 3], ALU.subtract, ["gtmp", "valsP"], ["gtmp"])
                CP("dve", valsP[:, :, 4], gtmp[:], ["gtmp", "valsP"], ["valsP"])

            def comp_build(e_, j):
                si = j % 6
                TS("dve", Sj[si][:, :], iota_s[:, 0:514], pos[:, e_, j:j + 1], msk[:, e_, j:j + 1],
                   ALU.is_equal, ALU.mult, ["iota_s", "pos", "msk"], [f"Sj{si}"])

            def comp_mm(e_, j):
                si = j % 6
                MM(B[5][:, 0:257], valsP[:, j, :], Sj[si][:, 0:257], j == 0, j == NT - 1, [f"Sj{si}", "valsP"], ["B5"])
                MM(B[6][:, 0:257], valsP[:, j, :], Sj[si][:, 257:514], j == 0, j == NT - 1, [f"Sj{si}", "valsP"], ["B6"])

            def comp_finalize(e_, par):
                CP("act", cv[0:32, 0:257], B[5][0:32, 0:257], ["B5"], ["cv"])
                CP("act", cv[0:32, 257:514], B[6][0:32, 0:257], ["B6", "cv"], ["cv"])
                b4v = B[4][:].rearrange("p (s n) -> p s n", s=4)
                for st in range(4):
                    s0_ = SLOT_TILES[st][0]
                    TR(b4v[:, st, :], cv[:, s0_:s0_ + 128], identf[:], ["cv", "identf"], ["B4"])
                s0_ = SLOT_TILES[4][0]
                TR(B[3][:, 0:128], cv[:, s0_:s0_ + 128], identf[:], ["cv", "identf"], ["B3"])
                CP("dve", idxv[:, 0:4, :], b4v[:, :, 0:5], ["B4"], ["idxv"])
                CP("dve", idxv[:, 4, :], B[3][:, 0:5], ["B3", "idxv"], ["idxv"])
                STT(idxf[:], idxv[:, :, 0], 64.0, idxv[:, :, 1], ALU.mult, ALU.add, ["idxv"], ["idxf"])
                TS("dve", idxi[par][:], idxf[:], 0.25, None, ALU.add, None, ["idxf"], [f"idxi{par}"])
                TT("dve", gat[par][:], idxv[:, :, 2], idxv[:, :, 3], ALU.add, ["idxv"], [f"gat{par}"])
                TT("dve", gat[par][:], gat[par][:], idxv[:, :, 4], ALU.add, ["idxv", f"gat{par}"], [f"gat{par}"])
                if debug:
                    CP("dve", dbgi[:, e_ * 10:e_ * 10 + 5], idxf[:], ["idxf"], ["dbgi"])
                    CP("dve", dbgi[:, e_ * 10 + 5:e_ * 10 + 10], gat[par][:], [f"gat{par}", "dbgi"], ["dbgi"])

            def gather(e_, par):
                for st, (s0, ns) in enumerate(SLOT_TILES):
                    P.dma("pool", lambda e, st=st, ns=ns, par=par: e.indirect_dma_start(
                        out=xg[0:ns, st, :], out_offset=None, in_=n2d,
                        in_offset=bass.IndirectOffsetOnAxis(ap=idxi[par][0:ns, st:st + 1], axis=0)),
                        f"gath{st}", [f"idxi{par}", "n2d0"], [f"xg{st}"])

            BT2 = B[6][:].bitcast(BF16).rearrange("p (c n) -> p c n", c=8)

            def transposes(e_):
                for st, (s0, ns) in enumerate(SLOT_TILES):
                    bt, btk = (BT[:, :, :], "BT") if st % 2 == 0 else (BT2, "B6")
                    for cc in range(8):
                        TR(bt[:, cc, :], xg[:, st, cc * 128:(cc + 1) * 128], identb[:, :],
                           [f"xg{st}", "identb"], [btk])
                    TT("dve", xgT[:, :, s0:s0 + ns], bt[:, :, 0:ns],
                       gffn_col[:, :].unsqueeze(2).to_broadcast([128, 8, ns]), ALU.mult, [btk, "gffn_col"], ["xgT"])

            comp_prepare(0)
            for j in range(NT):
                comp_build(0, j)
                comp_mm(0, j)
            comp_finalize(0, 0)
            gather(0, 0)
            transposes(0)
            SP = [(0, 512), (512, 512), (1024, 512), (1536, 512), (2048, 512), (2560, 256)]
            NSPE = len(SP)
            NSP = NE * NSPE

            def chunk_src(K, i):
                ee, pp = divmod(K, NSPE)
                col0, pw = SP[pp]
                which, ch = divmod(i, 2)
                src = (w_gate, w_up)[which][ee].rearrange("(c p) n -> p c n", p=128)
                return src[:, 4 * ch:4 * ch + 4, col0:col0 + pw], pw, which, ch

            def gu_dma(K, i):
                if K >= NSP:
                    return
                src, pw, which, ch = chunk_src(K, i)
                sv = stg[i][:, 0:4 * pw].rearrange("p (c n) -> p c n", c=4)
                DMA("sp", sv, src, f"stg{i}", [], [f"stg{i}"])

            def gu_cast(K, i):
                if K >= NSP:
                    return
                src, pw, which, ch = chunk_src(K, i)
                sv = stg[i][:, 0:4 * pw].rearrange("p (c n) -> p c n", c=4)
                wdst, wkey = ((Wg[K % 2], f"Wg{K % 2}"), (Wu[K % 2], f"Wu{K % 2}"))[which]
                CP("act", wdst[:, 4 * ch:4 * ch + 4, 0:pw], sv, [f"stg{i}"], [wkey])

            for i in range(4):
                gu_dma(0, i)
            for i in range(4):
                gu_cast(0, i)
                gu_dma(1, i)
            acc_keys = [f"accsc{st}_{half}" for st in range(5) for half in range(2)]
            for e_ in range(NE):
                par = e_ % 2
                nxt = e_ + 1 < NE
                if nxt:
                    comp_prepare(e_ + 1)
                wgv = w_gate[e_].rearrange("(c p) n -> p c n", p=128)
                wuv = w_up[e_].rearrange("(c p) n -> p c n", p=128)
                wdv = w_down[e_].rearrange("(f p) n -> p f n", p=128)
                for pp, (col0, pw) in enumerate(SP):
                    K = e_ * NSPE + pp
                    wsl = K % 2
                    jlist = list(range(6 * pp, min(6 * pp + 6, NT)))
                    if nxt:
                        for j in jlist:
                            comp_build(e_ + 1, j)
                    nfc = pw // 128
                    ngroups = 2 * nfc
                    cast_after = {1: 0, 3: 1, 5: 2, 7: 3} if ngroups == 8 else {0: 0, 1: 1, 2: 2, 3: 3}
                    gi = 0
                    for fl in range(nfc):
                        f = col0 // 128 + fl
                        for half in range(2):
                            c0 = half * 257
                            bg = bankc[0] % 5
                            bu = (bankc[0] + 1) % 5
                            bankc[0] += 2
                            for (wt_, wkey, bb) in ((Wg[wsl], f"Wg{wsl}", bg), (Wu[wsl], f"Wu{wsl}", bu)):
                                for cc in range(8):
                                    MM(B[bb][:, 0:257], wt_[:, cc, fl * 128:(fl + 1) * 128], xgT[:, cc, c0:c0 + 257],
                                       cc == 0, cc == 7, [wkey, "xgT"], [f"B{bb}"])
                            si = sgc[0] % 2
                            sgc[0] += 1
                            ACT(sg[si][:, :], B[bg][:, 0:257], AF.Silu, [f"B{bg}"], [f"sg{si}"])
                            TT("dve", hidT[:, f, c0:c0 + 257], sg[si][:, :], B[bu][:, 0:257], ALU.mult,
                               [f"sg{si}", f"B{bu}"], ["hidT"])
                            if gi in cast_after:
                                ci = cast_after[gi]
                                gu_cast(K + 1, ci)
                                gu_dma(K + 2, ci)
                            if half == 1:
                                ws = wdc[0] % 2
                                wdc[0] += 1
                                DMA("pool", stgW[ws][:, :], wdv[:, f, :], f"stgW{ws}", [], [f"stgW{ws}"])
                                CP("dve", Wd[:, f, :], stgW[ws][:, :], [f"stgW{ws}"], ["Wd"])
                            gi += 1
                    if nxt:
                        for j in jlist:
                            comp_mm(e_ + 1, j)
                P.wait_all("pool", acc_keys)
                for half in range(2):
                    if half == 1 and nxt:
                        comp_finalize(e_ + 1, 1 - par)
                        gather(e_ + 1, 1 - par)
                    dbank = [(st + 5 * half) % 7 for st in range(5)]
                    for f in range(NF):
                        for st, (s0, ns) in enumerate(SLOT_TILES):
                            MM(B[dbank[st]][:, 0:512], hidT[:, f, s0:s0 + 128], Wd[:, f, half * 512:(half + 1) * 512],
                               f == 0, f == NF - 1, ["hidT", "Wd"], [f"B{dbank[st]}"])
                    for st, (s0, ns) in enumerate(SLOT_TILES):
                        yi = ytc[0] % 5
                        ytc[0] += 1
                        TS("dve", yt[yi][0:ns, :], B[dbank[st]][0:ns, 0:512], gat[par][0:ns, st:st + 1], None, ALU.mult, None,
                           [f"B{dbank[st]}", f"gat{par}"], [f"yt{yi}"])
                        P.dma("pool", lambda e, st=st, ns=ns, yi=yi, half=half, par=par: e.indirect_dma_start(
                            out=accs[half], out_offset=bass.IndirectOffsetOnAxis(ap=idxi[par][0:ns, st:st + 1], axis=0),
                            in_=yt[yi][0:ns, :], in_offset=None, compute_op=ALU.add),
                            f"scat{st}_{half}", [f"yt{yi}", f"idxi{par}", "acc_init0", "acc_init1"], [f"accsc{st}_{half}"])
                if nxt:
                    transposes(e_ + 1)
            if debug:
                DMA("sp", dbg_idx, dbgi[:], "dbgidx", ["dbgi"], ["dbg_idx"])


            P.barrier()
            gfin = stgW[0][:, 0:D]
            hf = [stg[0][:, 0:D], stg[0][:, D:2 * D], stg[1][:, 0:D], stg[1][:, D:2 * D]]
            of = [stg[2][:, 0:D], stg[2][:, D:2 * D], stg[3][:, 0:D], stg[3][:, D:2 * D]]
            sqf = stgW[1][:, 0:D]
            ssq3 = sb("ssq3", [128, 1], F32, s2)
            rstd3 = sb("rstd3", [128, 1], F32, s2)
            DMA("sp", gfin[:], gfin_rep_d, "gfin", [], ["gfin"])
            out_keys = []
            def fin_load(j):
                sl = j % 4
                for half in range(2):
                    DMA("sp", hf[sl][:, half * 512:(half + 1) * 512], accs[half][j * 128:(j + 1) * 128, :], f"hf{sl}",
                        acc_keys + ["acc_init0", "acc_init1"], [f"hf{sl}"])

            for j in range(3):
                fin_load(j)
            for j in range(NT):
                sl = j % 4
                ACT(sqf[:], hf[sl][:], AF.Square, [f"hf{sl}"], ["sqf", "ssq3"], accum=ssq3[:])
                ACT(rstd3[:], ssq3[:], AF.Ln, ["ssq3", "epsc"], ["rstd3"], bias=epsc[:], scale=1.0 / D)
                ACT(rstd3[:], rstd3[:], AF.Exp, ["rstd3"], ["rstd3"], scale=-0.5)
                STT(of[sl][:], hf[sl][:], rstd3[:], gfin[:], ALU.mult, ALU.mult, [f"hf{sl}", "rstd3", "gfin"], [f"of{sl}"])
                if j + 3 < NT:
                    fin_load(j + 3)
                if j == 0:
                    DMA("sp", out[0:112, :], of[sl][16:128, :], f"of{sl}", [f"of{sl}"], [f"out{sl}"])
                elif j == NT - 1:
                    DMA("sp", out[4080:4096, :], of[sl][0:16, :], f"of{sl}", [f"of{sl}"], [f"out{sl}"])
                else:
                    DMA("sp", out[j * 128 - 16:j * 128 + 112, :], of[sl][:, :], f"of{sl}", [f"of{sl}"], [f"out{sl}"])
            keys = ["out0", "out1", "out2", "out3"] + (["dbg_h1", "dbg_aff", "dbg_idx"] if debug else [])
            P.wait_all("sp", keys)
            P.emit()
    return nc


def _consts():
    p = np.arange(128, dtype=np.float32)[:, None]
    c = {}
    c["identf"] = np.eye(128, dtype=np.float32)
    c["identb"] = np.eye(128).astype(ml_dtypes.bfloat16)
    c["onesf"] = np.ones((128, 128), np.float32)
    c["onesb"] = np.ones((128, 128)).astype(ml_dtypes.bfloat16)
    ol = np.zeros((128, 128), np.float32)
    ol[:L - 128 * (NT - 1)] = 1.0
    c["onesl"] = ol.astype(ml_dtypes.bfloat16)
    c["ltri"] = (np.arange(128)[:, None] < np.arange(128)[None, :]).astype(np.float32)
    f = np.arange(512, dtype=np.float32)[None, :]
    c["rstrip"] = (f - p).astype(np.float32)
    y = np.arange(896, dtype=np.float32)[None, :]
    c["astrip"] = (-np.abs(y - p - 384.0)).astype(np.float32)
    c["iota_slots"] = np.broadcast_to(np.arange(640, dtype=np.float32)[None, :], (128, 640)).copy()
    j = np.arange(NT, dtype=np.float32)[None, :]
    tok = (128.0 * j + p).astype(np.float32)
    c["tokidx"] = tok
    c["toka"] = np.floor(tok / 64.0).astype(np.float32)
    c["tokb"] = (tok - 64.0 * np.floor(tok / 64.0)).astype(np.float32)
    valid = (tok < L).astype(np.float32)
    c["validm"] = valid
    c["validm1"] = (valid - 1.0).astype(np.float32)
    corr = np.ones((128, 4, 16), np.float32)
    for gi, w in enumerate((2, 4, 8, 16)):
        for t in range(8):
            cnt = min(t + w // 2, L) - max(t - w // 2, 0)
            corr[:, gi, t] = w / cnt
            tt = L - 8 + t
            cnt = min(tt + w // 2, L) - max(tt - w // 2, 0)
            corr[:, gi, 8 + t] = w / cnt
    c["pool_corr"] = corr
    return c


_NC_CACHE = {}


def _get_nc(debug=False):
    if debug not in _NC_CACHE:
        _NC_CACHE[debug] = build(debug)
    return _NC_CACHE[debug]


def _prep_inputs(x, meta_tokens, g_mix, w_in, lam_vecs, subln_gain, pool_w, pool_scale, w_o,
                 g_ffn, router_w, w_gate, w_up, w_down, g_final):
    f = lambda a: np.ascontiguousarray(np.asarray(a, dtype=np.float32))
    x = f(x)
    meta = f(meta_tokens)
    shared = dict(_consts())
    shared["w_in"] = f(w_in)[0]
    shared["w_o"] = f(w_o)[0]
    shared["router_w"] = f(router_w)[0]
    shared["pool_w"] = f(pool_w)[0]
    shared["w_gate"] = f(w_gate)[0]
    shared["w_up"] = f(w_up)[0]
    shared["w_down"] = f(w_down)[0]
    shared["gmix_col"] = np.ascontiguousarray(f(g_mix)[0].reshape(8, 128).T)
    shared["gffn_col"] = np.ascontiguousarray(f(g_ffn)[0].reshape(8, 128).T)
    shared["gfin_rep"] = np.ascontiguousarray(np.broadcast_to(f(g_final)[None, :], (128, D)))
    shared["lam_rep"] = np.ascontiguousarray(np.broadcast_to(f(lam_vecs)[0].reshape(1, 256), (128, 256)))
    shared["subln_col"] = np.ascontiguousarray(f(subln_gain)[0].reshape(128, 1))
    shared["pscale_col"] = np.ascontiguousarray(f(pool_scale)[0].reshape(4, 128).T)
    in_maps = []
    for core in range(8):
        b = core // 2
        hx = np.zeros((LP, D), np.float32)
        hx[:16] = meta
        hx[16:L] = x[b]
        m = dict(shared)
        m["hx"] = hx
        in_maps.append(m)
    return in_maps


def kernel(x, meta_tokens, g_mix, w_in, lam_vecs, subln_gain, pool_w, pool_scale, w_o,
           g_ffn, router_w, w_gate, w_up, w_down, g_final):
    in_maps = _prep_inputs(x, meta_tokens, g_mix, w_in, lam_vecs, subln_gain, pool_w, pool_scale, w_o,
                           g_ffn, router_w, w_gate, w_up, w_down, g_final)
    nc = _get_nc(False)
    res = run_bass_kernel_spmd(nc, in_maps, core_ids=list(range(8)))
    outs = [np.asarray(res.results[2 * b]["out"], dtype=np.float32) for b in range(4)]
    return np.stack(outs, axis=0)
```

```python
from contextlib import ExitStack
import math
import numpy as np
import ml_dtypes
import concourse.bass as bass
import concourse.mybir as mybir
from concourse.bass_utils import run_bass_kernel_spmd

F32 = mybir.dt.float32
BF16 = mybir.dt.bfloat16
I32 = mybir.dt.int32
ALU = mybir.AluOpType
AF = mybir.ActivationFunctionType
AX = mybir.AxisListType

ENGS = ("pe", "dve", "act", "pool", "sp")

D = 1024
L = 4112
NT = 33
LP = NT * 128
NE = 16
CAP = 514
FF = 2816
NF = FF // 128
EPS = 1e-6
LAM_INIT = 0.2
SLOT_TILES = [(0, 2), (2, 128), (130, 128), (258, 128), (386, 128)]
BISECT_ITERS = 26


class Prog:
    def __init__(self, nc):
        self.nc = nc
        self.q = {e: [] for e in ENGS}
        self.cnt = {e: 0 for e in ENGS}
        self.waited = {e: {} for e in ENGS}
        self.last_w = {}
        self.readers = {}
        self.dcnt = {}
        self.sems = {}

    def sem(self, name):
        if name not in self.sems:
            self.sems[name] = self.nc.alloc_semaphore(name="s_" + name)
        return self.sems[name]

    def _deps(self, eng, reads, writes, skip_self):
        deps = set()
        for k in reads:
            if k in self.last_w:
                deps.add(self.last_w[k])
        for k in writes:
            if k in self.last_w:
                deps.add(self.last_w[k])
            deps.update(self.readers.get(k, ()))
        best = {}
        for (s, v) in deps:
            if skip_self and s == eng:
                continue
            if v > best.get(s, 0):
                best[s] = v
        waits = []
        for s in sorted(best):
            v = best[s]
            if self.waited[eng].get(s, 0) >= v:
                continue
            self.waited[eng][s] = v
            waits.append((s, v))
        return waits

    def _commit(self, me, reads, writes):
        for k in writes:
            self.last_w[k] = me
            self.readers[k] = []
        for k in reads:
            if k not in writes:
                self.readers.setdefault(k, []).append(me)

    def op(self, eng, fn, reads=(), writes=(), skip_self=False):
        waits = self._deps(eng, reads, writes, skip_self)
        self.cnt[eng] += 1
        me = (eng, self.cnt[eng])
        self.q[eng].append((waits, fn, eng, 1))
        self._commit(me, reads, writes)
        return me

    def dma(self, eng, fn, semname, reads=(), writes=()):
        waits = self._deps(eng, reads, writes, False)
        semname = "d_" + semname
        self.dcnt[semname] = self.dcnt.get(semname, 0) + 16
        me = (semname, self.dcnt[semname])
        self.q[eng].append((waits, fn, semname, 16))
        self._commit(me, reads, writes)
        return me

    def finalize_group(self, semname, keys):
        me = ("d_" + semname, self.dcnt["d_" + semname])
        for k in keys:
            self.last_w[k] = me

    def barrier(self):
        snap = {e: self.cnt[e] for e in ENGS if self.cnt[e] > 0}
        snap.update(self.dcnt)
        for eng in ENGS:
            waits = []
            for s in sorted(snap):
                v = snap[s]
                if s == eng or self.waited[eng].get(s, 0) >= v:
                    continue
                self.waited[eng][s] = v
                waits.append((s, v))
            if waits:
                self.q[eng].append((waits, None, None, 0))

    def wait_all(self, eng, keys):
        waits = self._deps(eng, keys, keys, False)
        if waits:
            self.q[eng].append((waits, None, None, 0))

    def emit(self):
        nc = self.nc
        for e in ENGS:
            self.sem(e)
        for s in list(self.dcnt):
            self.sem(s)
        prog = self

        def run(engname, engine):
            for waits, fn, incsem, incv in prog.q[engname]:
                for (s, v) in waits:
                    engine.wait_ge(prog.sem(s), v)
                if fn is not None:
                    inst = fn(engine)
                    inst.then_inc(prog.sem(incsem), incv)

        with nc.Block() as block:
            @block.tensor
            def _(eng):
                run("pe", eng)

            @block.vector
            def _(eng):
                run("dve", eng)

            @block.scalar
            def _(eng):
                run("act", eng)

            @block.gpsimd
            def _(eng):
                run("pool", eng)

            @block.sync
            def _(eng):
                run("sp", eng)


def build(debug=False):
    nc = bass.Bass("TRN2", target_bir_lowering=False)
    P = Prog(nc)

    def din(name, shape, dt=F32):
        return nc.dram_tensor(name, list(shape), dt, kind="ExternalInput").ap()

    hx = din("hx", [LP, D])
    w_in = din("w_in", [D, 2048])
    w_o = din("w_o", [D, D])
    router_w = din("router_w", [D, NE])
    pool_w = din("pool_w", [4, 128, 128])
    w_gate = din("w_gate", [NE, D, FF])
    w_up = din("w_up", [NE, D, FF])
    w_down = din("w_down", [NE, FF, D])
    gmix_col_d = din("gmix_col", [128, 8])
    gffn_col_d = din("gffn_col", [128, 8])
    gfin_rep_d = din("gfin_rep", [128, D])
    lam_rep_d = din("lam_rep", [128, 256])
    subln_col_d = din("subln_col", [128, 1])
    pscale_col_d = din("pscale_col", [128, 4])
    identf_d = din("identf", [128, 128])
    identb_d = din("identb", [128, 128], BF16)
    onesf_d = din("onesf", [128, 128])
    onesb_d = din("onesb", [128, 128], BF16)
    onesl_d = din("onesl", [128, 128], BF16)
    ltri_d = din("ltri", [128, 128])
    rstrip_d = din("rstrip", [128, 512])
    astrip_d = din("astrip", [128, 896])
    iota_d = din("iota_slots", [128, 640])
    tokidx_d = din("tokidx", [128, NT])
    toka_d = din("toka", [128, NT])
    tokb_d = din("tokb", [128, NT])
    validm_d = din("validm", [128, NT])
    vm1_d = din("validm1", [128, NT])
    corr_d = din("pool_corr", [128, 4, 16])

    out = nc.dram_tensor("out", [4096, D], F32, kind="ExternalOutput").ap()
    acc0 = nc.dram_tensor("acc0", [LP, 512], F32, kind="Internal").ap()
    acc1 = nc.dram_tensor("acc1", [LP, 512], F32, kind="Internal").ap()
    accs = [acc0, acc1]
    n2d = nc.dram_tensor("n2d", [LP, D], BF16, kind="Internal").ap()
    if debug:
        dbg_h1 = nc.dram_tensor("dbg_h1", [LP, D], F32, kind="ExternalOutput").ap()
        dbg_aff = nc.dram_tensor("dbg_aff", [128, NT * NE], F32, kind="ExternalOutput").ap()
        dbg_idx = nc.dram_tensor("dbg_idx", [128, NE * 5 * 2], F32, kind="ExternalOutput").ap()

    def MM(o, lhsT, rhs, start, stop, r, w):
        P.op("pe", lambda e: e.matmul(o, lhsT=lhsT, rhs=rhs, start=start, stop=stop), r, w, skip_self=True)

    def TR(o, i, ident, r, w):
        P.op("pe", lambda e: e.transpose(out=o, in_=i, identity=ident), r, w, skip_self=True)

    def ACT(o, i, func, r, w, bias=None, scale=None, accum=None):
        kw = {}
        if bias is not None:
            kw["bias"] = bias
        if scale is not None:
            kw["scale"] = scale
        if accum is not None:
            kw["accum_out"] = accum
        P.op("act", lambda e: e.activation(out=o, in_=i, func=func, **kw), r, w)

    def TS(eng, o, i0, s1, s2, op0, op1, r, w):
        if op1 is None:
            P.op(eng, lambda e: e.tensor_scalar(out=o, in0=i0, scalar1=s1, scalar2=None, op0=op0), r, w)
        else:
            P.op(eng, lambda e: e.tensor_scalar(out=o, in0=i0, scalar1=s1, scalar2=s2, op0=op0, op1=op1), r, w)

    def TT(eng, o, i0, i1, op, r, w):
        P.op(eng, lambda e: e.tensor_tensor(out=o, in0=i0, in1=i1, op=op), r, w)

    def STT(o, i0, sc, i1, op0, op1, r, w):
        P.op("dve", lambda e: e.scalar_tensor_tensor(out=o, in0=i0, scalar=sc, in1=i1, op0=op0, op1=op1), r, w)

    def CP(eng, o, i, r, w):
        if eng == "act":
            P.op("act", lambda e: e.activation(out=o, in_=i, func=AF.Copy), r, w)
        else:
            P.op(eng, lambda e: e.tensor_copy(out=o, in_=i), r, w)

    def DMA(eng, o, i, sem, r, w):
        P.dma(eng, lambda e: e.dma_start(out=o, in_=i), sem, r, w)

    with ExitStack() as g:
        def sb(name, shape, dt=F32, es=g):
            return es.enter_context(nc.sbuf_tensor("sb_" + name, list(shape), dt))

        def ps(name, shape, dt=F32, es=g):
            return es.enter_context(nc.psum_tensor("ps_" + name, list(shape), dt))

        s1 = ExitStack()
        identf = sb("identf", [128, 128])
        identb = sb("identb", [128, 128], BF16)
        onesf = sb("onesf", [128, 128])
        onesb = sb("onesb", [128, 128], BF16)
        ltri = sb("ltri", [128, 128])
        iota_s = sb("iota_s", [128, 640])
        tokidx = sb("tokidx", [128, NT])
        toka = sb("toka", [128, NT])
        tokb = sb("tokb", [128, NT])
        gffn_col = sb("gffn_col", [128, 8])
        epsc = sb("epsc", [128, 1])
        neglam = sb("neglam", [128, 1])
        gcol8 = sb("gcol8", [128, 1])
        lam2 = sb("lam2", [128, 2])
        aff_all = sb("aff_all", [128, NT, NE])

        onesl = sb("onesl", [128, 128], BF16, s1)
        rstrip = sb("rstrip", [128, 512], F32, s1)
        astrip = sb("astrip", [128, 896], F32, s1)
        validm = sb("validm", [128, NT], F32, s1)
        vm1 = sb("vm1", [128, NT], F32, s1)
        corr = sb("corr", [128, 4, 16], F32, s1)
        gmix_col = sb("gmix_col", [128, 8], F32, s1)
        lam_rep = sb("lam_rep", [128, 256], F32, s1)
        subln_col = sb("subln_col", [128, 1], F32, s1)
        pscale_col = sb("pscale_col", [128, 4], F32, s1)
        rw_f = sb("rw_f", [128, 8, NE], F32, s1)
        rw_g = sb("rw_g", [128, 8, NE], F32, s1)
        pw_f = sb("pw_f", [128, 4, 128], F32, s1)
        pw_b = sb("pw_b", [128, 4, 128], BF16, s1)
        lamt = sb("lamt", [128, 2, 64], F32, s1)
        consts = [(identf, identf_d, "identf"), (identb, identb_d, "identb"), (onesf, onesf_d, "onesf"),
                  (onesb, onesb_d, "onesb"), (onesl, onesl_d, "onesl"), (ltri, ltri_d, "ltri"), (rstrip, rstrip_d, "rstrip"),
                  (astrip, astrip_d, "astrip"), (iota_s, iota_d, "iota_s"), (tokidx, tokidx_d, "tokidx"), (toka, toka_d, "toka"), (tokb, tokb_d, "tokb"),
                  (validm, validm_d, "validm"), (vm1, vm1_d, "vm1"), (corr, corr_d, "corr"),
                  (gmix_col, gmix_col_d, "gmix_col"), (gffn_col, gffn_col_d, "gffn_col"),
                  (lam_rep, lam_rep_d, "lam_rep"), (subln_col, subln_col_d, "subln_col"),
                  (pscale_col, pscale_col_d, "pscale_col"),
                  (rw_f, router_w.rearrange("(c p) n -> p c n", p=128), "rw_f"),
                  (pw_f, pool_w.rearrange("g c d -> c g d"), "pw_f")]
        for i, (t, d, k) in enumerate(consts):
            DMA("act" if i % 2 else "sp", t[:], d, "const", [], [k])
        P.finalize_group("const", [k for (_, _, k) in consts])

        P.op("pool", lambda e: e.memset(epsc[:], EPS), [], ["epsc"])
        lam4 = lam_rep[:].rearrange("p (i w k) -> p i w k", i=2, w=2)
        TT("dve", lamt[:], lam4[:, :, 0, :], lam4[:, :, 1, :], ALU.mult, ["lam_rep"], ["lamt"])
        P.op("dve", lambda e: e.tensor_reduce(out=lam2[:], in_=lamt[:], axis=AX.X, op=ALU.add), ["lamt"], ["lam2"])
        ACT(lam2[:], lam2[:], AF.Exp, ["lam2"], ["lam2"])
        TT("dve", neglam[:], lam2[:, 1:2], lam2[:, 0:1], ALU.subtract, ["lam2"], ["neglam"])
        TS("dve", neglam[:], neglam[:], -LAM_INIT, None, ALU.add, None, ["neglam"], ["neglam"])
        TS("dve", gcol8[:], subln_col[:], 1.0 - LAM_INIT, None, ALU.mult, None, ["subln_col"], ["gcol8"])
        CP("dve", pw_b[:], pw_f[:], ["pw_f"], ["pw_b"])
        for c in range(8):
            TS("dve", rw_g[:, c, :], rw_f[:, c, :], gffn_col[:, c:c + 1], None, ALU.mult, None,
               ["rw_f", "gffn_col"], ["rw_g"])

        gmix_b8 = gmix_col[:, :].unsqueeze(2).to_broadcast([128, 8, 128])
        with s1:
            KT = sb("KT", [128, 4, LP], BF16, s1)
            V = sb("V", [128, NT, 512], BF16, s1)
            pT = sb("pT", [128, 4, LP], BF16, s1)
            xt = [sb(f"xt{i}", [128, D], F32, s1) for i in range(2)]
            nt = sb("nt", [128, D], F32, s1)
            nT = sb("nT", [128, 8, 512], BF16, s1)
            sqj = sb("sqj", [128, D], BF16, s1)
            ssq = sb("ssq", [128, 1], F32, s1)
            rstd = sb("rstd", [128, 1], F32, s1)
            RB = [ps(f"RB{i}", [128, 512], F32, s1) for i in range(4)]
            psO = [ps(f"psO{i}", [128, 512], F32, s1) for i in range(4)]
            xcnt = [0]
            rbc = [0]

            def rb():
                i = rbc[0] % 4
                rbc[0] += 1
                return RB[i], f"RB{i}"

            ntb = [nt[:, 0:512].bitcast(BF16), nt[:, 512:1024].bitcast(BF16)]
            ssqs = [sb(f"ssqs{i}", [128, 1], F32, s1) for i in range(2)]
            rstds = [sb(f"rstds{i}", [128, 1], F32, s1) for i in range(2)]
            ncnt = [0]

            def norm_tile(j, ntok_off):
                sl = xcnt[0] % 2
                xcnt[0] += 1
                nb = ncnt[0] % 2
                ncnt[0] += 1
                nk = f"ntb{nb}"
                DMA("sp", xt[sl][:], hx[j * 128:(j + 1) * 128, :], f"xt{sl}", [], [f"xt{sl}"])
                ACT(sqj[:], xt[sl][:], AF.Square, [f"xt{sl}"], ["sqj", f"ssqs{nb}"], accum=ssqs[nb][:])
                ACT(rstds[nb][:], ssqs[nb][:], AF.Ln, [f"ssqs{nb}", "epsc"], [f"rstds{nb}"], bias=epsc[:], scale=1.0 / D)
                ACT(rstds[nb][:], rstds[nb][:], AF.Exp, [f"rstds{nb}"], [f"rstds{nb}"], scale=-0.5)
                TS("dve", ntb[nb], xt[sl][:], rstds[nb][:], None, ALU.mult, None, [f"xt{sl}", f"rstds{nb}"], [nk, "nt"])
                bank, bk = rb()
                pv = bank[:].bitcast(BF16).rearrange("p (c n) -> p c n", c=8)
                for cc in range(8):
                    TR(pv[:, cc, :], ntb[nb][:, cc * 128:(cc + 1) * 128], identb[:], [nk, "identb"], [bk])
                CP("act" if nb else "dve", nT[:, :, ntok_off:ntok_off + 128], pv, [bk], ["nT"])

            castc = [0]

            def load_cast(dst, dkey, src_view, col0, ncols, stg, stgk, fold=None):
                for i in range(ncols // 256):
                    sl = i % 2
                    sv = stg[sl][:].rearrange("p (c n) -> p c n", c=8)
                    DMA("sp", sv, src_view[:, :, col0 + 256 * i: col0 + 256 * (i + 1)], stgk[sl], [], [stgk[sl]])
                    if fold is None:
                        CP("act" if castc[0] % 2 else "dve", dst[:, :, 256 * i:256 * (i + 1)], sv, [stgk[sl]], [dkey])
                        castc[0] += 1
                    else:
                        for c in range(8):
                            if c % 2:
                                P.op("act", lambda e, c=c, i=i, sv=sv: e.activation(
                                    out=dst[:, c, 256 * i:256 * (i + 1)], in_=sv[:, c, :], func=AF.Copy,
                                    scale=fold[:, c:c + 1]), [stgk[sl], "gmix_col"], [dkey])
                            else:
                                TS("dve", dst[:, c, 256 * i:256 * (i + 1)], sv[:, c, :], fold[:, c:c + 1], None, ALU.mult,
                                   None, [stgk[sl], "gmix_col"], [dkey])

            w_in_v = w_in.rearrange("(c p) n -> p c n", p=128)
            with ExitStack() as sa:
                wkvu = sb("wkvu", [128, 8, 1536], BF16, sa)
                stgA = [sb(f"stgA{i}", [128, 2048], F32, sa) for i in range(2)]
                UT = [sb(f"UT{i}", [128, 4, 528], F32, sa) for i in range(2)]
                tA = sb("tA", [128, 528], F32, sa)
                tB = sb("tB", [128, 528], F32, sa)
                tC = sb("tC", [128, 528], F32, sa)
                pooled4 = [sb(f"pooled{i}", [128, 512], BF16, sa) for i in range(4)]
                pool_pe = []
                load_cast(wkvu, "wkvu", w_in_v, 512, 1536, stgA, ["stgA0", "stgA1"], fold=gmix_col)
                P.op("pool", lambda e: e.memset(UT[0][:], 0.0), [], ["UT0"])
                P.op("pool", lambda e: e.memset(UT[1][:], 0.0), [], ["UT1"])
                nchunks = 9
                psrot = [0]

                def nextbank():
                    b = psrot[0] % 8
                    psrot[0] += 1
                    if b < 4:
                        return RB[b], f"RB{b}"
                    return psO[b - 4], f"psO{b - 4}"

                def do_pool(c):
                    u = UT[c % 2]
                    uk = f"UT{c % 2}"
                    nq = 512 if c < 8 else 128
                    E = "dve"
                    for gi, w in enumerate((2, 4, 8, 16)):
                        ug = u[:, gi, :]
                        if gi == 0:
                            TT(E, tC[:, 0:512], ug[:, 7:519], ug[:, 8:520], ALU.add, [uk], ["tC"])
                        elif gi == 1:
                            TT(E, tA[:, 0:526], ug[:, 0:526], ug[:, 1:527], ALU.add, [uk], ["tA"])
                            TT(E, tC[:, 0:512], tA[:, 6:518], tA[:, 8:520], ALU.add, ["tA"], ["tC"])
                        elif gi == 2:
                            TT(E, tA[:, 0:526], ug[:, 0:526], ug[:, 1:527], ALU.add, [uk], ["tA"])
                            TT(E, tB[:, 0:524], tA[:, 0:524], tA[:, 2:526], ALU.add, ["tA"], ["tB"])
                            TT(E, tC[:, 0:512], tB[:, 4:516], tB[:, 8:520], ALU.add, ["tB"], ["tC"])
                        else:
                            TT(E, tA[:, 0:526], ug[:, 0:526], ug[:, 1:527], ALU.add, [uk], ["tA"])
                            TT(E, tB[:, 0:524], tA[:, 0:524], tA[:, 2:526], ALU.add, ["tA"], ["tB"])
                            TT(E, tA[:, 0:520], tB[:, 0:520], tB[:, 4:524], ALU.add, ["tB"], ["tA"])
                            TT(E, tC[:, 0:512], tA[:, 0:512], tA[:, 8:520], ALU.add, ["tA"], ["tC"])
                        win = tC[:, 0:512]
                        wk = "tC"
                        if c == 0:
                            TT(E, tC[:, 0:8], tC[:, 0:8], corr[:, gi, 0:8], ALU.mult, [wk, "corr"], [wk])
                        if c == 8:
                            TT(E, tC[:, 8:16], tC[:, 8:16], corr[:, gi, 8:16], ALU.mult, [wk, "corr"], [wk])
                        pooled = pooled4[gi]
                        STT(pooled[:, :], win, 1.0 / w, ug[:, 8:520], ALU.mult, ALU.subtract, [wk, uk], [f"pooled{gi}"])

                        def pe_part(gi=gi, c=c, nq=nq, pooled=pooled):
                            bank, bk = nextbank()
                            MM(bank[:, 0:nq], pw_b[:, gi, :], pooled[:, 0:nq], True, True, ["pw_b", f"pooled{gi}"], [bk])
                            P.op("act", lambda e: e.activation(
                                out=pT[:, gi, c * 512:c * 512 + nq], in_=bank[:, 0:nq], func=AF.Copy,
                                scale=pscale_col[:, gi:gi + 1]), [bk, "pscale_col"], ["pT"])
                        pool_pe.append(pe_part)

                for c in range(nchunks):
                    ntl = 4 if c < 8 else 1
                    nq = ntl * 128
                    for ti in range(ntl):
                        norm_tile(c * 4 + ti, ti * 128)
                    for fc in range(4):
                        bank, bk = nextbank()
                        for kc in range(8):
                            MM(bank[:, 0:nq], wkvu[:, kc, fc * 128:(fc + 1) * 128], nT[:, kc, 0:nq], kc == 0, kc == 7,
                               ["wkvu", "nT"], [bk])
                        CP("act", KT[:, fc, c * 512:c * 512 + nq], bank[:, 0:nq], [bk], ["KT"])
                    while pool_pe:
                        pool_pe.pop(0)()
                    u = UT[c % 2]
                    uk = f"UT{c % 2}"
                    if c >= 2:
                        P.op("pool", lambda e, u=u: e.memset(u[:], 0.0), [], [uk])
                    for gi in range(4):
                        bank, bk = nextbank()
                        for kc in range(8):
                            MM(bank[:, 0:nq], wkvu[:, kc, 1024 + gi * 128:1024 + (gi + 1) * 128], nT[:, kc, 0:nq],
                               kc == 0, kc == 7, ["wkvu", "nT"], [bk])
                        CP("act" if gi % 2 else "dve", u[:, gi, 8:8 + nq], bank[:, 0:nq], [bk], [uk])
                    for ti in range(ntl):
                        bank, bk = nextbank()
                        for kc in range(8):
                            MM(bank[:, 0:512], nT[:, kc, ti * 128:(ti + 1) * 128], wkvu[:, kc, 512:1024], kc == 0, kc == 7,
                               ["wkvu", "nT"], [bk])
                        CP("act" if ti % 2 else "dve", V[:, c * 4 + ti, :], bank[:, 0:512], [bk], ["V"])
                    if c >= 1:
                        up = UT[(c - 1) % 2]
                        upk = f"UT{(c - 1) % 2}"
                        CP("dve", up[:, :, 520:528], u[:, :, 8:16], [uk], [upk])
                        CP("dve", u[:, :, 0:8], up[:, :, 512:520], [upk], [uk])
                        do_pool(c - 1)
                while pool_pe:
                    pool_pe.pop(0)()
                do_pool(nchunks - 1)
                while pool_pe:
                    pool_pe.pop(0)()

            P.barrier()
            with ExitStack() as sq_:
                wq = sb("wq", [128, 8, 512], BF16, sq_)
                wo = sb("wo", [128, 8, D], BF16, sq_)
                with ExitStack() as stmp:
                    stgB = [sb(f"stgB{i}", [128, 2048], F32, stmp) for i in range(2)]
                    load_cast(wq, "wq", w_in_v, 0, 512, stgB, ["stgB0", "stgB1"], fold=gmix_col)
                    load_cast(wo, "wo", w_o.rearrange("(c p) n -> p c n", p=128), 0, 1024, stgB, ["stgB0", "stgB1"])
                P.barrier()
                NTMP = 3
                NPT = 5
                PD = 3
                QT = sb("QT", [128, 4, 2, 512], BF16, sq_)
                P.op("pool", lambda e: e.memset(QT[:], 0.0), [], ["QT"])
                tmp = [sb(f"tmp{i}", [128, 512], F32, sq_) for i in range(NTMP)]
                PT = [sb(f"PT{i}", [128, 512], BF16, sq_) for i in range(NPT)]
                eA = sb("eA", [128, 512], F32, sq_)
                eB = sb("eB", [128, 512], F32, sq_)
                eD = sb("eD", [128, 512], F32, sq_)
                eS = sb("eS", [128, 512], F32, sq_)
                eR = eS
                aTs = [sb(f"aT{i}", [128, 4, 512], BF16, sq_) for i in range(2)]
                h1t = sb("h1t", [128, D], F32, sq_)
                n2f = nt
                n2b = [sqj]
                n2T = sb("n2T", [128, 8, 128], F32, sq_)
                ssq2 = sb("ssq2", [128, 1], F32, sq_)
                rstd2 = sb("rstd2", [128, 1], F32, sq_)
                lg = sb("lg", [128, NE], F32, sq_)
                mx = sb("mx", [128, 1], F32, sq_)
                se = sb("se", [128, 1], F32, sq_)
                tcount = [0]
                hcount = [0]

                pending_b = [None]

                def step_b():
                    if pending_b[0] is not None:
                        try:
                            next(pending_b[0])
                        except StopIteration:
                            pending_b[0] = None

                def drain_b():
                    while pending_b[0] is not None:
                        step_b()

                def phase_b(c, q0, ntl, aT, aTk):
                    for ti in range(ntl):
                        j = c * 4 + ti
                        tok = slice(ti * 128, (ti + 1) * 128)
                        sl = xcnt[0] % 2
                        xcnt[0] += 1
                        DMA("sp", xt[sl][:], hx[j * 128:(j + 1) * 128, :], f"xt{sl}", [], [f"xt{sl}"])
                        for half in range(2):
                            bank, bk = rb()
                            for mc in range(8):
                                if mc < 4:
                                    lh = aT[:, mc, tok]
                                    rk = aTk
                                else:
                                    lh = pT[:, mc - 4, q0 + ti * 128:q0 + (ti + 1) * 128]
                                    rk = "pT"
                                MM(bank[:, :], lh, wo[:, mc, half * 512:(half + 1) * 512],
                                   mc == 0, mc == 7, [rk, "wo"], [bk])
                            TT("dve", h1t[:, half * 512:(half + 1) * 512], bank[:, :], xt[sl][:, half * 512:(half + 1) * 512],
                               ALU.add, [bk, f"xt{sl}"], ["h1t"])
                            yield
                        for half in range(2):
                            DMA("sp", accs[half][j * 128:(j + 1) * 128, :], h1t[:, half * 512:(half + 1) * 512],
                                f"h1st{half}", ["h1t"], [f"acc_init{half}"])
                        if debug:
                            DMA("sp", dbg_h1[j * 128:(j + 1) * 128, :], h1t[:], "dbgh1", ["h1t"], ["dbg_h1"])
                        ACT(sqj[:], h1t[:], AF.Square, ["h1t"], ["sqj", "ssq2"], accum=ssq2[:])
                        ACT(rstd2[:], ssq2[:], AF.Ln, ["ssq2", "epsc"], ["rstd2"], bias=epsc[:], scale=1.0 / D)
                        ACT(rstd2[:], rstd2[:], AF.Exp, ["rstd2"], ["rstd2"], scale=-0.5)
                        yield
                        TS("dve", n2f[:], h1t[:], rstd2[:], None, ALU.mult, None, ["h1t", "rstd2", "ntb0", "ntb1"], ["nt", "ntb0", "ntb1"])
                        yield
                        CP("act", n2b[0][:], n2f[:], ["nt"], ["sqj"])
                        DMA("sp", n2d[j * 128:(j + 1) * 128, :], n2b[0][:], "n2st0", ["sqj"], ["n2d0"])
                        for hb in range(2):
                            bank, bk = rb()
                            pv = bank[:].rearrange("p (c n) -> p c n", c=4)
                            for cc in range(4):
                                c8 = hb * 4 + cc
                                TR(pv[:, cc, :], n2f[:, c8 * 128:(c8 + 1) * 128], identf[:], ["nt", "identf"], [bk])
                            CP("act" if hb else "dve", n2T[:, hb * 4:hb * 4 + 4, :], pv, [bk], ["n2T"])
                            yield
                        bank, bk = rb()
                        for cc in range(8):
                            MM(bank[:, 0:NE], n2T[:, cc, :], rw_g[:, cc, :], cc == 0, cc == 7, ["n2T", "rw_g"], [bk])
                        P.op("dve", lambda e, bank=bank: e.tensor_reduce(out=mx[:], in_=bank[:, 0:NE], axis=AX.X, op=ALU.max),
                             [bk], ["mx"])
                        TS("dve", mx[:], mx[:], -1.0, None, ALU.mult, None, ["mx"], ["mx"])
                        ACT(lg[:], bank[:, 0:NE], AF.Exp, [bk, "mx"], ["lg", "se"], bias=mx[:], accum=se[:])
                        yield
                        P.op("dve", lambda e: e.reciprocal(out=se[:], in_=se[:]), ["se"], ["se"])
                        TS("dve", lg[:], lg[:], se[:], None, ALU.mult, None, ["lg", "se"], ["lg"])
                        STT(aff_all[:, j, :], lg[:], validm[:, j:j + 1], vm1[:, j:j + 1].to_broadcast([128, NE]),
                            ALU.mult, ALU.add, ["lg", "validm", "vm1"], ["aff_all"])
                        yield

                for c in range(9):
                    ntl = 4 if c < 8 else 1
                    nq = ntl * 128 if c < 8 else L - 8 * 512
                    q0 = c * 512
                    for ti in range(ntl):
                        norm_tile(c * 4 + ti, ti * 128)
                    for fc in range(4):
                        bank, bk = rb()
                        for kc in range(8):
                            MM(bank[:, 0:nq], wq[:, kc, fc * 128:(fc + 1) * 128], nT[:, kc, 0:nq], kc == 0, kc == 7,
                               ["wq", "nT"], [bk])
                        ACT(QT[0:64, fc, 0, 0:nq], bank[0:64, 0:nq], AF.Copy, [bk], ["QT"], scale=0.125)
                        ACT(QT[64:128, fc, 1, 0:nq], bank[64:128, 0:nq], AF.Copy, [bk, "QT"], ["QT"], scale=0.125)
                    tiles = []
                    for h in range(4):
                        m = 2.0 ** (-2.0 * (h + 1))
                        for mp in range(2):
                            kbs = []
                            for kb in range(NT):
                                k0 = kb * 128
                                kp = 128 if kb < 32 else 16
                                dist = max(0, k0 - (q0 + nq - 1), q0 - (k0 + kp - 1))
                                if dist * m > 32.0:
                                    continue
                                kbs.append(kb)
                            for ii, kb in enumerate(kbs):
                                tiles.append(dict(h=h, mp=mp, kb=kb, first=(ii == 0), last=(ii == len(kbs) - 1), m=m))
                    deferred = []

                    def emit_S(T, nq=nq, q0=q0):
                        h, mp, kb, m = T["h"], T["mp"], T["kb"], T["m"]
                        hp, base = h // 2, 64 * (h % 2)
                        fcq = mp * 2 + hp
                        k0 = kb * 128
                        kp = 128
                        t = tcount[0]
                        tcount[0] += 1
                        T["t"] = t
                        sS, sk = rb()
                        tm, tk = tmp[t % NTMP], f"tmp{t % NTMP}"
                        pt, pk = PT[t % NPT], f"PT{t % NPT}"
                        MM(sS[0:kp, 0:nq], KT[:, fcq, k0:k0 + kp], QT[:, fcq, h % 2, 0:nq],
                           True, True, ["KT", "QT"], [sk])
                        Dd = q0 - k0
                        if k0 + kp - 1 < q0:
                            STT(tm[0:kp, 0:nq], rstrip[0:kp, 0:nq], -m, sS[0:kp, 0:nq], ALU.mult, ALU.add,
                                ["rstrip", sk], [tk])
                            cb = -m * Dd
                        elif k0 >= q0 + nq:
                            STT(tm[0:kp, 0:nq], rstrip[0:kp, 0:nq], m, sS[0:kp, 0:nq], ALU.mult, ALU.add,
                                ["rstrip", sk], [tk])
                            cb = m * Dd
                        else:
                            STT(tm[0:kp, 0:nq], astrip[0:kp, Dd + 384:Dd + 384 + nq], m, sS[0:kp, 0:nq],
                                ALU.mult, ALU.add, ["astrip", sk], [tk])
                            cb = 0.0
                        ACT(pt[0:kp, 0:nq], tm[0:kp, 0:nq], AF.Exp, [tk], [pk], bias=(float(cb) if cb != 0.0 else None))

                    aT = aTs[c % 2]
                    aTk = f"aT{c % 2}"

                    seqc = [0]

                    def defer(time, cls, fn, writes_eD=False, mp=None):
                        seqc[0] += 1
                        deferred.append(dict(time=time, cls=cls, seq=seqc[0], fn=fn, weD=writes_eD, mp=mp))
                        deferred.sort(key=lambda d: (d["time"], d["seq"]))

                    def run_item(d):
                        if d not in deferred:
                            return
                        for o in [o for o in deferred if o is not d and ((o["cls"] == d["cls"] and o["seq"] < d["seq"])
                                                                       or (d["weD"] and o["cls"] == "tail"))]:
                            run_item(o)
                        if d in deferred:
                            deferred.remove(d)
                            d["fn"]()

                    def run_deferred(it, flush=False):
                        while deferred and (flush or deferred[0]["time"] <= it):
                            run_item(deferred[0])

                    def start_tail(h, it, nq=nq, aT=aT, aTk=aTk):
                        bankbox = {}

                        def stC():
                            ACT(eS[:, 0:nq], eD[:, 0:nq], AF.Square, ["eD"], ["eS"])

                        def stD():
                            bank, bk = rb()
                            bankbox["b"] = (bank, bk)
                            MM(bank[:, 0:nq], onesf[:], eS[:, 0:nq], True, True, ["onesf", "eS"], [bk])
                            ACT(eR[:, 0:nq], bank[:, 0:nq], AF.Ln, [bk, "epsc"], ["eS"], bias=epsc[:], scale=1.0 / 128)

                        def stE():
                            ACT(eR[:, 0:nq], eR[:, 0:nq], AF.Exp, ["eS"], ["eS"], scale=-0.5)

                        def stF():
                            STT(aT[:, h, 0:nq], eD[:, 0:nq], gcol8[:], eR[:, 0:nq], ALU.mult, ALU.mult,
                                ["eD", "gcol8", "eS"], [aTk])

                        defer(it + 2, "tail", stC)
                        defer(it + 4, "tail", stD)
                        defer(it + 6, "tail", stE)
                        defer(it + 9, "tail", stF)

                    def start_norm(h, mp, it, nq=nq):
                        def stA():
                            ACT(eA[:, 0:nq], psO[2 + mp][:, 0:nq], AF.Ln, [f"psO{2 + mp}"], ["eA"])
                            ACT(eA[:, 0:nq], eA[:, 0:nq], AF.Exp, ["eA"], ["eA"], scale=-1.0)

                        def stB():
                            if mp == 0:
                                TT("dve", eB[:, 0:nq], psO[0][:, 0:nq], eA[:, 0:nq], ALU.mult, ["psO0", "eA"], ["eB"])
                            else:
                                TT("dve", eA[:, 0:nq], psO[1][:, 0:nq], eA[:, 0:nq], ALU.mult, ["psO1", "eA"], ["eA"])
                                STT(eD[:, 0:nq], eA[:, 0:nq], neglam[:], eB[:, 0:nq], ALU.mult, ALU.add,
                                    ["eA", "neglam", "eB"], ["eD"])
                                start_tail(h, max(it + 8, curit[0]))

                        defer(it + 5, "norm", stA, mp=mp)
                        defer(it + 8, "norm", stB, writes_eD=(mp == 1), mp=mp)

                    curit = [0]

                    def emit_AV(T, it, nq=nq):
                        h, mp, kb = T["h"], T["mp"], T["kb"]
                        kp = 128
                        t = T["t"]
                        pt, pk = PT[t % NPT], f"PT{t % NPT}"
                        if T["first"]:
                            for d in [d for d in deferred if d["cls"] == "norm" and d["mp"] == mp]:
                                run_item(d)
                        MM(psO[mp][:, 0:nq], V[0:kp, kb, h * 128:(h + 1) * 128], pt[0:kp, 0:nq],
                           T["first"], T["last"], ["V", pk], [f"psO{mp}"])
                        MM(psO[2 + mp][:, 0:nq], (onesl if kb == NT - 1 else onesb)[0:kp, :], pt[0:kp, 0:nq],
                           T["first"], T["last"], ["onesb", "onesl", pk], [f"psO{2 + mp}"])
                        if T["last"]:
                            start_norm(h, mp, it)

                    ntile = len(tiles)
                    for it in range(ntile + PD):
                        if it < ntile:
                            emit_S(tiles[it])
                        curit[0] = it
                        if it >= PD:
                            emit_AV(tiles[it - PD], it)
                        run_deferred(it)
                        if it % 3 == 2:
                            step_b()
                    run_deferred(10 ** 9, flush=True)

                    drain_b()
                    pending_b[0] = phase_b(c, q0, ntl, aT, aTk)
                drain_b()
        P.barrier()
        with ExitStack() as s2:
            msk = sb("msk", [128, NE, NT], F32, s2)
            pos = sb("pos", [128, NE, NT], F32, s2)
            lo = sb("lo", [128, NE], F32, s2)
            cmpb = sb("cmpb", [128, NE, NT], F32, s2)
            cntp = sb("cntp", [128, NE], F32, s2)
            gsel = sb("gsel", [128, NE], F32, s2)
            basep = sb("basep", [128, NE], F32, s2)
            onesc = sb("onesc", [128, NT], F32, s2)
            B = [ps(f"B{i}", [128, 512], F32, s2) for i in range(7)]
            BT = ps("BT", [128, 8, 128], BF16, s2)
            affT = aff_all[:].rearrange("p j e -> p e j")

            P.op("dve", lambda e: e.memset(lo[:], 0.0), [], ["lo"])
            P.op("dve", lambda e: e.memset(onesc[:], 1.0), [], ["onesc"])
            for it in range(BISECT_ITERS):
                step = 2.0 ** (-(it + 1))
                STT(cmpb[:], affT, step, lo[:].unsqueeze(2).to_broadcast([128, NE, NT]), ALU.subtract, ALU.is_ge,
                    ["aff_all", "lo"], ["cmpb"])
                P.op("dve", lambda e: e.tensor_reduce(out=cntp[:], in_=cmpb[:], axis=AX.X, op=ALU.add), ["cmpb"], ["cntp"])
                MM(B[0][:, 0:NE], onesf[:], cntp[:], True, True, ["onesf", "cntp"], ["B0"])
                TS("dve", gsel[:], B[0][:, 0:NE], CAP - 0.5, step, ALU.is_ge, ALU.mult, ["B0"], ["gsel"])
                TT("dve", lo[:], lo[:], gsel[:], ALU.add, ["lo", "gsel"], ["lo"])
            TT("dve", msk[:], affT, lo[:].unsqueeze(2).to_broadcast([128, NE, NT]), ALU.is_ge, ["aff_all", "lo"], ["msk"])
            P.op("dve", lambda e: e.tensor_reduce(out=cntp[:], in_=msk[:], axis=AX.X, op=ALU.add), ["msk"], ["cntp"])
            MM(B[0][:, 0:NE], ltri[:], cntp[:], True, True, ["ltri", "cntp"], ["B0"])
            CP("dve", basep[:], B[0][:, 0:NE], ["B0"], ["basep"])
            for e_ in range(NE):
                P.op("dve", lambda e, e_=e_: e.tensor_tensor_scan(out=pos[:, e_, :], data0=onesc[:], data1=msk[:, e_, :],
                                                                  initial=0.0, op0=ALU.mult, op1=ALU.add),
                     ["onesc", "msk"], ["pos"])
            TT("dve", pos[:], pos[:], msk[:], ALU.subtract, ["pos", "msk"], ["pos"])
            TT("dve", pos[:], pos[:], basep[:].unsqueeze(2).to_broadcast([128, NE, NT]), ALU.add, ["pos", "basep"], ["pos"])
            if debug:
                DMA("sp", dbg_aff, aff_all[:].rearrange("p j e -> p (j e)"), "dbgaff", ["aff_all"], ["dbg_aff"])

            valsP = sb("valsP", [128, NT, 128], BF16, s2)
            gtmp = sb("gtmp", [128, NT], F32, s2)
            Sj = [sb(f"Sj{i}", [128, 514], BF16, s2) for i in range(6)]
            cv = sb("cv", [128, 516], F32, s2)
            idxv = sb("idxv", [128, 5, 5], F32, s2)
            idxf = sb("idxf", [128, 5], F32, s2)
            idxi = [sb(f"idxi{i}", [128, 5], I32, s2) for i in range(2)]
            gat = [sb(f"gat{i}", [128, 5], F32, s2) for i in range(2)]
            xg = sb("xg", [128, 5, D], BF16, s2)
            xgT = sb("xgT", [128, 8, 516], BF16, s2)
            hidT = sb("hidT", [128, NF, 516], BF16, s2)
            Wd = sb("Wd", [128, NF, D], BF16, s2)
            stg = [sb(f"stg{i}", [128, 2048], F32, s2) for i in range(4)]
            stgW = [sb(f"stgW{i}", [128, 1024], F32, s2) for i in range(2)]
            wdc = [0]
            Wg = [sb(f"Wg{i}", [128, 8, 512], BF16, s2) for i in range(2)]
            Wu = [sb(f"Wu{i}", [128, 8, 512], BF16, s2) for i in range(2)]
            sg = [sb(f"sg{i}", [128, 257], F32, s2) for i in range(2)]
            yt = [sb(f"yt{i}", [128, 512], F32, s2) for i in range(5)]
            stgc = [0]
            wpc = [0]
            bankc = [0]
            sgc = [0]
            ytc = [0]
            ssc = [0]
            xg_keys = ["xg0", "xg1", "xg2", "xg3", "xg4"]
            if debug:
                dbgi = sb("dbgi", [128, NE * 5 * 2], F32, s2)

            P.op("pool", lambda e: e.memset(valsP[:], 0.0), [], ["valsP"])
            P.op("pool", lambda e: e.memset(cv[:], 0.0), [], ["cv"])
            P.op("pool", lambda e: e.memset(xg[:], 0.0), [], xg_keys)
            P.op("pool", lambda e: e.memset(hidT[:], 0.0), [], ["hidT"])
            CP("dve", valsP[:, :, 0], toka[:], ["toka", "valsP"], ["valsP"])
            CP("dve", valsP[:, :, 1], tokb[:], ["tokb", "valsP"], ["valsP"])

            def comp_prepare(e_):
                g = aff_all[:, :, e_]
                CP("dve", valsP[:, :, 2], g, ["aff_all", "valsP"], ["valsP"])
                TT("dve", gtmp[:], g, valsP[:, :, 2], ALU.subtract, ["aff_all", "valsP"], ["gtmp"])
                CP("dve", valsP[:, :, 3], gtmp[:], ["gtmp", "valsP"], ["valsP"])
                TT("dve", gtmp[:], gtmp[:], valsP[:, :, 3], ALU.subtract, ["gtmp", "valsP"], ["gtmp"])
                CP("dve", valsP[:, :, 4], gtmp[:], ["gtmp", "valsP"], ["valsP"])

            def comp_build(e_, j):
                si = j % 6
                TS("dve", Sj[si][:, :], iota_s[:, 0:514], pos[:, e_, j:j + 1], msk[:, e_, j:j + 1],
                   ALU.is_equal, ALU.mult, ["iota_s", "pos", "msk"], [f"Sj{si}"])

            def comp_mm(e_, j):
                si = j % 6
                MM(B[5][:, 0:257], valsP[:, j, :], Sj[si][:, 0:257], j == 0, j == NT - 1, [f"Sj{si}", "valsP"], ["B5"])
                MM(B[6][:, 0:257], valsP[:, j, :], Sj[si][:, 257:514], j == 0, j == NT - 1, [f"Sj{si}", "valsP"], ["B6"])

            def comp_finalize(e_, par):
                CP("act", cv[0:32, 0:257], B[5][0:32, 0:257], ["B5"], ["cv"])
                CP("act", cv[0:32, 257:514], B[6][0:32, 0:257], ["B6", "cv"], ["cv"])
                b4v = B[4][:].rearrange("p (s n) -> p s n", s=4)
                for st in range(4):
                    s0_ = SLOT_TILES[st][0]
                    TR(b4v[:, st, :], cv[:, s0_:s0_ + 128], identf[:], ["cv", "identf"], ["B4"])
                s0_ = SLOT_TILES[4][0]
                TR(B[3][:, 0:128], cv[:, s0_:s0_ + 128], identf[:], ["cv", "identf"], ["B3"])
                CP("dve", idxv[:, 0:4, :], b4v[:, :, 0:5], ["B4"], ["idxv"])
                CP("dve", idxv[:, 4, :], B[3][:, 0:5], ["B3", "idxv"], ["idxv"])
                STT(idxf[:], idxv[:, :, 0], 64.0, idxv[:, :, 1], ALU.mult, ALU.add, ["idxv"], ["idxf"])
                TS("dve", idxi[par][:], idxf[:], 0.25, None, ALU.add, None, ["idxf"], [f"idxi{par}"])
                TT("dve", gat[par][:], idxv[:, :, 2], idxv[:, :, 3], ALU.add, ["idxv"], [f"gat{par}"])
                TT("dve", gat[par][:], gat[par][:], idxv[:, :, 4], ALU.add, ["idxv", f"gat{par}"], [f"gat{par}"])
                if debug:
                    CP("dve", dbgi[:, e_ * 10:e_ * 10 + 5], idxf[:], ["idxf"], ["dbgi"])
                    CP("dve", dbgi[:, e_ * 10 + 5:e_ * 10 + 10], gat[par][:], [f"gat{par}", "dbgi"], ["dbgi"])

            def gather(e_, par):
                for st, (s0, ns) in enumerate(SLOT_TILES):
                    P.dma("pool", lambda e, st=st, ns=ns, par=par: e.indirect_dma_start(
                        out=xg[0:ns, st, :], out_offset=None, in_=n2d,
                        in_offset=bass.IndirectOffsetOnAxis(ap=idxi[par][0:ns, st:st + 1], axis=0)),
                        f"gath{st}", [f"idxi{par}", "n2d0"], [f"xg{st}"])

            BT2 = B[6][:].bitcast(BF16).rearrange("p (c n) -> p c n", c=8)

            def transposes(e_):
                for st, (s0, ns) in enumerate(SLOT_TILES):
                    bt, btk = (BT[:, :, :], "BT") if st % 2 == 0 else (BT2, "B6")
                    for cc in range(8):
                        TR(bt[:, cc, :], xg[:, st, cc * 128:(cc + 1) * 128], identb[:, :],
                           [f"xg{st}", "identb"], [btk])
                    TT("dve", xgT[:, :, s0:s0 + ns], bt[:, :, 0:ns],
                       gffn_col[:, :].unsqueeze(2).to_broadcast([128, 8, ns]), ALU.mult, [btk, "gffn_col"], ["xgT"])

            comp_prepare(0)
            for j in range(NT):
                comp_build(0, j)
                comp_mm(0, j)
            comp_finalize(0, 0)
            gather(0, 0)
            transposes(0)
            SP = [(0, 512), (512, 512), (1024, 512), (1536, 512), (2048, 512), (2560, 256)]
            NSPE = len(SP)
            NSP = NE * NSPE

            def chunk_src(K, i):
                ee, pp = divmod(K, NSPE)
                col0, pw = SP[pp]
                which, ch = divmod(i, 2)
                src = (w_gate, w_up)[which][ee].rearrange("(c p) n -> p c n", p=128)
                return src[:, 4 * ch:4 * ch + 4, col0:col0 + pw], pw, which, ch

            def gu_dma(K, i):
                if K >= NSP:
                    return
                src, pw, which, ch = chunk_src(K, i)
                sv = stg[i][:, 0:4 * pw].rearrange("p (c n) -> p c n", c=4)
                DMA("sp", sv, src, f"stg{i}", [], [f"stg{i}"])

            def gu_cast(K, i):
                if K >= NSP:
                    return
                src, pw, which, ch = chunk_src(K, i)
                sv = stg[i][:, 0:4 * pw].rearrange("p (c n) -> p c n", c=4)
                wdst, wkey = ((Wg[K % 2], f"Wg{K % 2}"), (Wu[K % 2], f"Wu{K % 2}"))[which]
                CP("act", wdst[:, 4 * ch:4 * ch + 4, 0:pw], sv, [f"stg{i}"], [wkey])

            for i in range(4):
                gu_dma(0, i)
            for i in range(4):
                gu_cast(0, i)
                gu_dma(1, i)
            acc_keys = [f"accsc{st}_{half}" for st in range(5) for half in range(2)]
            for e_ in range(NE):
                par = e_ % 2
                nxt = e_ + 1 < NE
                if nxt:
                    comp_prepare(e_ + 1)
                wgv = w_gate[e_].rearrange("(c p) n -> p c n", p=128)
                wuv = w_up[e_].rearrange("(c p) n -> p c n", p=128)
                wdv = w_down[e_].rearrange("(f p) n -> p f n", p=128)
                for pp, (col0, pw) in enumerate(SP):
                    K = e_ * NSPE + pp
                    wsl = K % 2
                    jlist = list(range(6 * pp, min(6 * pp + 6, NT)))
                    if nxt:
                        for j in jlist:
                            comp_build(e_ + 1, j)
                    nfc = pw // 128
                    ngroups = 2 * nfc
                    cast_after = {1: 0, 3: 1, 5: 2, 7: 3} if ngroups == 8 else {0: 0, 1: 1, 2: 2, 3: 3}
                    gi = 0
                    for fl in range(nfc):
                        f = col0 // 128 + fl
                        for half in range(2):
                            c0 = half * 257
                            bg = bankc[0] % 5
                            bu = (bankc[0] + 1) % 5
                            bankc[0] += 2
                            for (wt_, wkey, bb) in ((Wg[wsl], f"Wg{wsl}", bg), (Wu[wsl], f"Wu{wsl}", bu)):
                                for cc in range(8):
                                    MM(B[bb][:, 0:257], wt_[:, cc, fl * 128:(fl + 1) * 128], xgT[:, cc, c0:c0 + 257],
                                       cc == 0, cc == 7, [wkey, "xgT"], [f"B{bb}"])
                            si = sgc[0] % 2
                            sgc[0] += 1
                            ACT(sg[si][:, :], B[bg][:, 0:257], AF.Silu, [f"B{bg}"], [f"sg{si}"])
                            TT("dve", hidT[:, f, c0:c0 + 257], sg[si][:, :], B[bu][:, 0:257], ALU.mult,
                               [f"sg{si}", f"B{bu}"], ["hidT"])
                            if gi in cast_after:
                                ci = cast_after[gi]
                                gu_cast(K + 1, ci)
                                gu_dma(K + 2, ci)
                            if half == 1:
                                ws = wdc[0] % 2
                                wdc[0] += 1
                                DMA("pool", stgW[ws][:, :], wdv[:, f, :], f"stgW{ws}", [], [f"stgW{ws}"])
                                CP("dve", Wd[:, f, :], stgW[ws][:, :], [f"stgW{ws}"], ["Wd"])
                            gi += 1
                    if nxt:
                        for j in jlist:
                            comp_mm(e_ + 1, j)
                P.wait_all("pool", acc_keys)
                for half in range(2):
                    if half == 1 and nxt:
                        comp_finalize(e_ + 1, 1 - par)
                        gather(e_ + 1, 1 - par)
                    dbank = [(st + 5 * half) % 7 for st in range(5)]
                    for f in range(NF):
                        for st, (s0, ns) in enumerate(SLOT_TILES):
                            MM(B[dbank[st]][:, 0:512], hidT[:, f, s0:s0 + 128], Wd[:, f, half * 512:(half + 1) * 512],
                               f == 0, f == NF - 1, ["hidT", "Wd"], [f"B{dbank[st]}"])
                    for st, (s0, ns) in enumerate(SLOT_TILES):
                        yi = ytc[0] % 5
                        ytc[0] += 1
                        TS("dve", yt[yi][0:ns, :], B[dbank[st]][0:ns, 0:512], gat[par][0:ns, st:st + 1], None, ALU.mult, None,
                           [f"B{dbank[st]}", f"gat{par}"], [f"yt{yi}"])
                        P.dma("pool", lambda e, st=st, ns=ns, yi=yi, half=half, par=par: e.indirect_dma_start(
                            out=accs[half], out_offset=bass.IndirectOffsetOnAxis(ap=idxi[par][0:ns, st:st + 1], axis=0),
                            in_=yt[yi][0:ns, :], in_offset=None, compute_op=ALU.add),
                            f"scat{st}_{half}", [f"yt{yi}", f"idxi{par}", "acc_init0", "acc_init1"], [f"accsc{st}_{half}"])
                if nxt:
                    transposes(e_ + 1)
            if debug:
                DMA("sp", dbg_idx, dbgi[:], "dbgidx", ["dbgi"], ["dbg_idx"])


            P.barrier()
            gfin = stgW[0][:, 0:D]
            hf = [stg[0][:, 0:D], stg[0][:, D:2 * D], stg[1][:, 0:D], stg[1][:, D:2 * D]]
            of = [stg[2][:, 0:D], stg[2][:, D:2 * D], stg[3][:, 0:D], stg[3][:, D:2 * D]]
            sqf = stgW[1][:, 0:D]
            ssq3 = sb("ssq3", [128, 1], F32, s2)
            rstd3 = sb("rstd3", [128, 1], F32, s2)
            DMA("sp", gfin[:], gfin_rep_d, "gfin", [], ["gfin"])
            out_keys = []
            def fin_load(j):
                sl = j % 4
                for half in range(2):
                    DMA("sp", hf[sl][:, half * 512:(half + 1) * 512], accs[half][j * 128:(j + 1) * 128, :], f"hf{sl}",
                        acc_keys + ["acc_init0", "acc_init1"], [f"hf{sl}"])

            for j in range(3):
                fin_load(j)
            for j in range(NT):
                sl = j % 4
                ACT(sqf[:], hf[sl][:], AF.Square, [f"hf{sl}"], ["sqf", "ssq3"], accum=ssq3[:])
                ACT(rstd3[:], ssq3[:], AF.Ln, ["ssq3", "epsc"], ["rstd3"], bias=epsc[:], scale=1.0 / D)
                ACT(rstd3[:], rstd3[:], AF.Exp, ["rstd3"], ["rstd3"], scale=-0.5)
                STT(of[sl][:], hf[sl][:], rstd3[:], gfin[:], ALU.mult, ALU.mult, [f"hf{sl}", "rstd3", "gfin"], [f"of{sl}"])
                if j + 3 < NT:
                    fin_load(j + 3)
                if j == 0:
                    DMA("sp", out[0:112, :], of[sl][16:128, :], f"of{sl}", [f"of{sl}"], [f"out{sl}"])
                elif j == NT - 1:
                    DMA("sp", out[4080:4096, :], of[sl][0:16, :], f"of{sl}", [f"of{sl}"], [f"out{sl}"])
                else:
                    DMA("sp", out[j * 128 - 16:j * 128 + 112, :], of[sl][:, :], f"of{sl}", [f"of{sl}"], [f"out{sl}"])
            keys = ["out0", "out1", "out2", "out3"] + (["dbg_h1", "dbg_aff", "dbg_idx"] if debug else [])
            P.wait_all("sp", keys)
            P.emit()
    return nc


def _consts():
    p = np.arange(128, dtype=np.float32)[:, None]
    c = {}
    c["identf"] = np.eye(128, dtype=np.float32)
    c["identb"] = np.eye(128).astype(ml_dtypes.bfloat16)
    c["onesf"] = np.ones((128, 128), np.float32)
    c["onesb"] = np.ones((128, 128)).astype(ml_dtypes.bfloat16)
    ol = np.zeros((128, 128), np.float32)
    ol[:L - 128 * (NT - 1)] = 1.0
    c["onesl"] = ol.astype(ml_dtypes.bfloat16)
    c["ltri"] = (np.arange(128)[:, None] < np.arange(128)[None, :]).astype(np.float32)
    f = np.arange(512, dtype=np.float32)[None, :]
    c["rstrip"] = (f - p).astype(np.float32)
    y = np.arange(896, dtype=np.float32)[None, :]
    c["astrip"] = (-np.abs(y - p - 384.0)).astype(np.float32)
    c["iota_slots"] = np.broadcast_to(np.arange(640, dtype=np.float32)[None, :], (128, 640)).copy()
    j = np.arange(NT, dtype=np.float32)[None, :]
    tok = (128.0 * j + p).astype(np.float32)
    c["tokidx"] = tok
    c["toka"] = np.floor(tok / 64.0).astype(np.float32)
    c["tokb"] = (tok - 64.0 * np.floor(tok / 64.0)).astype(np.float32)
    valid = (tok < L).astype(np.float32)
    c["validm"] = valid
    c["validm1"] = (valid - 1.0).astype(np.float32)
    corr = np.ones((128, 4, 16), np.float32)
    for gi, w in enumerate((2, 4, 8, 16)):
        for t in range(8):
            cnt = min(t + w // 2, L) - max(t - w // 2, 0)
            corr[:, gi, t] = w / cnt
            tt = L - 8 + t
            cnt = min(tt + w // 2, L) - max(tt - w // 2, 0)
            corr[:, gi, 8 + t] = w / cnt
    c["pool_corr"] = corr
    return c


_NC_CACHE = {}


def _get_nc(debug=False):
    if debug not in _NC_CACHE:
        _NC_CACHE[debug] = build(debug)
    return _NC_CACHE[debug]


def _prep_inputs(x, meta_tokens, g_mix, w_in, lam_vecs, subln_gain, pool_w, pool_scale, w_o,
                 g_ffn, router_w, w_gate, w_up, w_down, g_final):
    f = lambda a: np.ascontiguousarray(np.asarray(a, dtype=np.float32))
    x = f(x)
    meta = f(meta_tokens)
    shared = dict(_consts())
    shared["w_in"] = f(w_in)[0]
    shared["w_o"] = f(w_o)[0]
    shared["router_w"] = f(router_w)[0]
    shared["pool_w"] = f(pool_w)[0]
    shared["w_gate"] = f(w_gate)[0]
    shared["w_up"] = f(w_up)[0]
    shared["w_down"] = f(w_down)[0]
    shared["gmix_col"] = np.ascontiguousarray(f(g_mix)[0].reshape(8, 128).T)
    shared["gffn_col"] = np.ascontiguousarray(f(g_ffn)[0].reshape(8, 128).T)
    shared["gfin_rep"] = np.ascontiguousarray(np.broadcast_to(f(g_final)[None, :], (128, D)))
    shared["lam_rep"] = np.ascontiguousarray(np.broadcast_to(f(lam_vecs)[0].reshape(1, 256), (128, 256)))
    shared["subln_col"] = np.ascontiguousarray(f(subln_gain)[0].reshape(128, 1))
    shared["pscale_col"] = np.ascontiguousarray(f(pool_scale)[0].reshape(4, 128).T)
    in_maps = []
    for core in range(8):
        b = core // 2
        hx = np.zeros((LP, D), np.float32)
        hx[:16] = meta
        hx[16:L] = x[b]
        m = dict(shared)
        m["hx"] = hx
        in_maps.append(m)
    return in_maps


def kernel(x, meta_tokens, g_mix, w_in, lam_vecs, subln_gain, pool_w, pool_scale, w_o,
           g_ffn, router_w, w_gate, w_up, w_down, g_final):
    in_maps = _prep_inputs(x, meta_tokens, g_mix, w_in, lam_vecs, subln_gain, pool_w, pool_scale, w_o,
                           g_ffn, router_w, w_gate, w_up, w_down, g_final)
    nc = _get_nc(False)
    res = run_bass_kernel_spmd(nc, in_maps, core_ids=list(range(8)))
    outs = [np.asarray(res.results[2 * b]["out"], dtype=np.float32) for b in range(4)]
    return np.stack(outs, axis=0)
```
